# Optimizing a Trainium2 kernel written in Bass

```python
import math
import jax
import jax.numpy as jnp
from jax import lax
import numpy as np

D_MODEL = 1024
BATCH = 4
SEQ = 8192
DEPTH = 2

CHUNK = 64
HG_HEADS = 4
HG_WIDTH = D_MODEL // 2
HG_DK = HG_WIDTH // HG_HEADS
RET_HEADS = 4
RET_WIDTH = D_MODEL // 4
RET_DK = RET_WIDTH // RET_HEADS
GDN_HEADS = 4
GDN_WIDTH = D_MODEL // 4
GDN_DK = GDN_WIDTH // GDN_HEADS
MIX_WIDTH = HG_WIDTH + RET_WIDTH + GDN_WIDTH
N_IN = 4 * HG_WIDTH + 4 * RET_WIDTH + 4 * GDN_WIDTH + 2 * GDN_HEADS
CONV_K = 4
ROPE_BASE = 10000.0
N_EXPERTS = 32
TOP_K = 4
D_FF_EXPERT = D_MODEL
SWIGLU_ALPHA = 1.702
SWIGLU_LIMIT = 7.0
EXPERT_BLOCK = 512
NORM_EPS = 1e-6
L2_EPS = 1e-6

kernel_name = 'hybrid_hgrn2_retnet_gdn_moe_adaln'


def rmsnorm(x, g):
    x32 = x.astype(jnp.float32)
    y = x32 * lax.rsqrt(jnp.mean(x32 * x32, axis=-1, keepdims=True) + NORM_EPS)
    return (y * g.astype(jnp.float32)).astype(x.dtype)


def l2norm(t):
    return t * lax.rsqrt(jnp.sum(t * t, axis=-1, keepdims=True) + L2_EPS)


def to_chunks(t):
    B, S, H = t.shape[:3]
    t = t.reshape((B, S // CHUNK, CHUNK, H) + t.shape[3:])
    return jnp.moveaxis(t, (1, 3), (0, 2))


def from_chunks(t):
    nC, B, H, C, d = t.shape
    return jnp.moveaxis(t, (0, 2), (1, 3)).reshape(B, nC * C, H, d)


def hgrn2_mix(q, f_logit, v, lb):
    log_f = jnp.logaddexp(jnp.log(lb), jnp.log1p(-lb) + jax.nn.log_sigmoid(f_logit))
    k = (1.0 - lb) * jax.nn.sigmoid(-f_logit)
    q = jax.nn.silu(q)
    qc, kc, vc, lfc = to_chunks(q), to_chunks(k), to_chunks(v), to_chunks(log_f)
    bc = jnp.cumsum(lfc, axis=-2)
    causal = jnp.tril(jnp.ones((CHUNK, CHUNK), bool))[:, :, None]

    def step(state, inp):
        q_, k_, v_, b_ = inp
        rel = jnp.exp(jnp.where(causal, b_[..., :, None, :] - b_[..., None, :, :], -jnp.inf))
        scores = jnp.einsum('bhtd,bhsd,bhtsd->bhts', q_, k_, rel)
        b_last = b_[..., -1:, :]
        o = scores @ v_ + jnp.einsum('bhtd,bhde->bhte', q_ * jnp.exp(b_), state)
        state = jnp.exp(b_last[..., 0, :])[..., None] * state + jnp.einsum('bhsd,bhse->bhde', k_ * jnp.exp(b_last - b_), v_)
        return state, o

    B, _, H, dk = q.shape
    s0 = jnp.zeros((B, H, dk, v.shape[-1]), jnp.float32)
    _, o = lax.scan(step, s0, (qc, kc, vc, bc))
    return from_chunks(o)


def rotary(t):
    S, d = t.shape[1], t.shape[-1]
    half = d // 2
    inv = ROPE_BASE ** (-jnp.linspace(0.0, 1.0, half, dtype=jnp.float32))
    ang = jnp.arange(S, dtype=jnp.float32)[:, None] * inv[None, :]
    cos, sin = jnp.cos(ang)[:, None, :], jnp.sin(ang)[:, None, :]
    t1, t2 = t[..., :half], t[..., half:]
    return jnp.concatenate([t1 * cos - t2 * sin, t1 * sin + t2 * cos], axis=-1)


def retention_mix(q, k, v):
    B, _, H, dk = q.shape
    q = rotary(q)
    k = rotary(k) * (dk ** -0.5)
    log_g = jnp.log1p(-jnp.exp2(-5.0 - jnp.arange(H, dtype=jnp.float32)))
    qc, kc, vc = to_chunks(q), to_chunks(k), to_chunks(v)
    j = jnp.arange(CHUNK, dtype=jnp.float32)
    causal = jnp.tril(jnp.ones((CHUNK, CHUNK), bool))
    decay = jnp.exp(jnp.where(causal, (j[:, None] - j[None, :])[None] * log_g[:, None, None], -jnp.inf))
    intra = jnp.einsum('nbhts,nbhse->nbhte', jnp.einsum('nbhtd,nbhsd->nbhts', qc, kc) * decay, vc)
    k_w = jnp.exp(log_g[:, None] * (CHUNK - 1.0 - j)[None, :])[:, :, None]
    kv = jnp.einsum('nbhsd,nbhse->nbhde', kc * k_w, vc)
    chunk_decay = jnp.exp(CHUNK * log_g)[:, None, None]

    def step(state, kv_n):
        return chunk_decay * state + kv_n, state

    _, s_prev = lax.scan(step, jnp.zeros((B, H, dk, v.shape[-1]), jnp.float32), kv)
    q_w = jnp.exp(log_g[:, None] * (j + 1.0)[None, :])[:, :, None]
    inter = jnp.einsum('nbhtd,nbhde->nbhte', qc * q_w, s_prev)
    return from_chunks(intra + inter)


def causal_depthwise_conv(u, w):
    return lax.conv_general_dilated(u, w[:, None, :], window_strides=(1,), padding=[(CONV_K - 1, 0)],
                                    dimension_numbers=('NWC', 'WIO', 'NWC'), feature_group_count=u.shape[-1])


def gated_delta_mix(qkv, a_logit, b_logit, A_log, dt_bias):
    B, S, _ = qkv.shape
    q, k, v = jnp.split(qkv, 3, axis=-1)
    q = l2norm(q.reshape(B, S, GDN_HEADS, GDN_DK)) * (GDN_DK ** -0.5)
    k = l2norm(k.reshape(B, S, GDN_HEADS, GDN_DK))
    v = v.reshape(B, S, GDN_HEADS, GDN_DK)
    beta = jax.nn.sigmoid(b_logit)
    g = -jnp.exp(A_log) * jax.nn.softplus(a_logit + dt_bias)
    qc, kc, vc = to_chunks(q), to_chunks(k), to_chunks(v)
    bc = to_chunks(beta)
    gc = jnp.cumsum(to_chunks(g), axis=-1)
    incl = jnp.tril(jnp.ones((CHUNK, CHUNK), bool))
    strict = jnp.tril(jnp.ones((CHUNK, CHUNK), bool), k=-1)
    rel = jnp.exp(jnp.where(incl, gc[..., :, None] - gc[..., None, :], -jnp.inf))
    kk = jnp.einsum('nbhtd,nbhsd->nbhts', kc, kc)
    m = jnp.where(strict, bc[..., :, None] * kk * rel, 0.0)
    rhs = jnp.concatenate([vc * bc[..., None], kc * (bc * jnp.exp(gc))[..., None]], axis=-1)
    sol = lax.linalg.triangular_solve(m, rhs, left_side=True, lower=True, unit_diagonal=True)
    w_val, k_cum = sol[..., :GDN_DK], sol[..., GDN_DK:]
    qk = jnp.einsum('nbhtd,nbhsd->nbhts', qc, kc) * rel
    q_dec = qc * jnp.exp(gc)[..., None]
    k_dec = kc * jnp.exp(gc[..., -1:] - gc)[..., None]
    last = jnp.exp(gc[..., -1])[..., None, None]

    def step(state, inp):
        w_, kcum_, qk_, qdec_, kdec_, last_ = inp
        v_new = w_ - kcum_ @ state
        o = qdec_ @ state + qk_ @ v_new
        state = last_ * state + jnp.swapaxes(kdec_, -1, -2) @ v_new
        return state, o

    s0 = jnp.zeros((B, GDN_HEADS, GDN_DK, GDN_DK), jnp.float32)
    _, o = lax.scan(step, s0, (w_val, k_cum, qk, q_dec, k_dec, last))
    return from_chunks(o)


def hybrid_mixer(h, w_in, w_out, hg_lb, hg_norm_g, ret_norm_g, gdn_conv_w, gdn_A_log, gdn_dt_bias, gdn_norm_g):
    B, S, _ = h.shape
    f32 = jnp.float32
    sizes = [HG_WIDTH] * 4 + [RET_WIDTH] * 4 + [3 * GDN_WIDTH, GDN_WIDTH, GDN_HEADS, GDN_HEADS]
    z = (h @ w_in).astype(f32)
    (hq, hf, hv, hgt, rq, rk, rv, rgt, gqkv, ggt, ga, gb) = jnp.split(z, np.cumsum(sizes)[:-1].tolist(), axis=-1)

    def heads(t, n):
        return t.reshape(B, S, n, -1)

    o_hg = hgrn2_mix(heads(hq, HG_HEADS), heads(hf, HG_HEADS), heads(hv, HG_HEADS), hg_lb)
    o_hg = rmsnorm(o_hg, hg_norm_g) * jax.nn.silu(heads(hgt, HG_HEADS))
    o_ret = retention_mix(heads(rq, RET_HEADS), heads(rk, RET_HEADS), heads(rv, RET_HEADS))
    o_ret = rmsnorm(o_ret, ret_norm_g) * jax.nn.silu(heads(rgt, RET_HEADS))
    qkv = jax.nn.silu(causal_depthwise_conv(gqkv, gdn_conv_w.astype(f32)))
    o_gdn = gated_delta_mix(qkv, ga, gb, gdn_A_log.astype(f32), gdn_dt_bias.astype(f32))
    o_gdn = rmsnorm(o_gdn, gdn_norm_g) * jax.nn.silu(heads(ggt, GDN_HEADS))
    o = jnp.concatenate([o_hg.reshape(B, S, HG_WIDTH), o_ret.reshape(B, S, RET_WIDTH),
                         o_gdn.reshape(B, S, GDN_WIDTH)], axis=-1)
    return o.astype(h.dtype) @ w_out


def moe_ffn(h, router_w, router_b, w1, b1, w2, b2):
    B, S, D = h.shape
    N = B * S
    NK = N * TOP_K
    xf = h.reshape(N, D)
    logits = (xf @ router_w + router_b).astype(jnp.float32)
    top_v, top_e = lax.top_k(logits, TOP_K)
    gates = jax.nn.softmax(top_v, axis=-1).astype(h.dtype)
    flat_e = top_e.reshape(NK)
    flat_tok = jnp.repeat(jnp.arange(N, dtype=jnp.int32), TOP_K)
    order = jnp.argsort(flat_e)
    se, stok, sg = flat_e[order], flat_tok[order], gates.reshape(NK)[order]
    counts = jax.ops.segment_sum(jnp.ones((NK,), jnp.int32), flat_e, num_segments=N_EXPERTS)
    padded = (counts + EXPERT_BLOCK - 1) // EXPERT_BLOCK * EXPERT_BLOCK
    pend = jnp.cumsum(padded)
    pstart = pend - padded
    cstart = jnp.cumsum(counts) - counts
    dest = pstart[se] + jnp.arange(NK, dtype=jnp.int32) - cstart[se]
    n_blocks = -(-NK // EXPERT_BLOCK) + N_EXPERTS
    P = n_blocks * EXPERT_BLOCK
    buf_tok = jnp.full((P,), N, jnp.int32).at[dest].set(stok)
    buf_gate = jnp.zeros((P,), h.dtype).at[dest].set(sg)
    block_e = jnp.minimum(jnp.searchsorted(pend, jnp.arange(n_blocks, dtype=jnp.int32) * EXPERT_BLOCK, side='right'),
                          N_EXPERTS - 1)
    xpad = jnp.concatenate([xf, jnp.zeros((1, D), xf.dtype)], axis=0)

    def expert_block(args):
        tok, e = args
        hid = xpad[tok] @ w1[e] + b1[e]
        x_glu = jnp.minimum(hid[:, :D_FF_EXPERT], SWIGLU_LIMIT)
        x_lin = jnp.clip(hid[:, D_FF_EXPERT:], -SWIGLU_LIMIT, SWIGLU_LIMIT)
        act = x_glu * jax.nn.sigmoid(SWIGLU_ALPHA * x_glu) * (x_lin + 1.0)
        return act @ w2[e] + b2[e]

    y = lax.map(expert_block, (buf_tok.reshape(n_blocks, EXPERT_BLOCK), block_e))
    y = y.reshape(P, D) * buf_gate[:, None]
    out = jax.ops.segment_sum(y, buf_tok, num_segments=N + 1)[:N]
    return out.reshape(B, S, D)


def setup_inputs(seed: int = 0) -> dict:
    key = jax.random.key(seed)
    ks = jax.random.split(key, 24)
    f32 = jnp.float32
    D = D_MODEL

    def nrm(k, shape, s):
        return jax.random.normal(k, shape, f32) * s

    dt = jnp.exp(jax.random.uniform(ks[13], (DEPTH, GDN_HEADS), f32, math.log(1e-3), math.log(1e-1)))
    return {
        'x': nrm(ks[0], (BATCH, SEQ, D), 1.0),
        'c': nrm(ks[1], (BATCH, D), 1.0),
        'ada_w': nrm(ks[2], (DEPTH, D, 6 * D), 0.5 * D ** -0.5),
        'ada_b': nrm(ks[3], (DEPTH, 6 * D), 0.02),
        'norm1_g': 1.0 + nrm(ks[4], (DEPTH, D), 0.02),
        'norm2_g': 1.0 + nrm(ks[5], (DEPTH, D), 0.02),
        'w_in': nrm(ks[6], (DEPTH, D, N_IN), D ** -0.5),
        'w_out': nrm(ks[7], (DEPTH, MIX_WIDTH, D), MIX_WIDTH ** -0.5),
        'hg_lb_logits': nrm(ks[8], (DEPTH, HG_WIDTH), 0.5),
        'hg_norm_g': 1.0 + nrm(ks[9], (DEPTH, HG_HEADS, HG_DK), 0.02),
        'ret_norm_g': 1.0 + nrm(ks[10], (DEPTH, RET_HEADS, RET_DK), 0.02),
        'gdn_conv_w': nrm(ks[11], (DEPTH, CONV_K, 3 * GDN_WIDTH), CONV_K ** -0.5),
        'gdn_A_log': jnp.log(jax.random.uniform(ks[12], (DEPTH, GDN_HEADS), f32, 1.0, 16.0)),
        'gdn_dt_bias': dt + jnp.log(-jnp.expm1(-dt)),
        'gdn_norm_g': 1.0 + nrm(ks[14], (DEPTH, GDN_HEADS, GDN_DK), 0.02),
        'router_w': nrm(ks[15], (DEPTH, D, N_EXPERTS), D ** -0.5),
        'router_b': nrm(ks[16], (DEPTH, N_EXPERTS), 0.01),
        'exp_w1': nrm(ks[17], (DEPTH, N_EXPERTS, D, 2 * D_FF_EXPERT), D ** -0.5),
        'exp_b1': nrm(ks[18], (DEPTH, N_EXPERTS, 2 * D_FF_EXPERT), 0.01),
        'exp_w2': nrm(ks[19], (DEPTH, N_EXPERTS, D_FF_EXPERT, D), D_FF_EXPERT ** -0.5),
        'exp_b2': nrm(ks[20], (DEPTH, N_EXPERTS, D), 0.01),
        'final_norm_g': 1.0 + nrm(ks[21], (D,), 0.02),
    }


def reference(x, c, ada_w, ada_b, norm1_g, norm2_g, w_in, w_out, hg_lb_logits, hg_norm_g, ret_norm_g,
              gdn_conv_w, gdn_A_log, gdn_dt_bias, gdn_norm_g, router_w, router_b, exp_w1, exp_b1, exp_w2,
              exp_b2, final_norm_g):
    lb = jnp.cumsum(jax.nn.softmax(hg_lb_logits.astype(jnp.float32), axis=0), axis=0)
    lb = jnp.maximum(lb - lb[0], 0.0)
    cond = jax.nn.silu(c)
    for l in range(DEPTH):
        mod = cond @ ada_w[l] + ada_b[l]
        sh1, sc1, gt1, sh2, sc2, gt2 = jnp.split(mod[:, None, :], 6, axis=-1)
        hn = rmsnorm(x, norm1_g[l]) * (1.0 + sc1) + sh1
        x = x + gt1 * hybrid_mixer(hn, w_in[l], w_out[l], lb[l].reshape(HG_HEADS, HG_DK), hg_norm_g[l],
                                   ret_norm_g[l], gdn_conv_w[l], gdn_A_log[l], gdn_dt_bias[l], gdn_norm_g[l])
        hn = rmsnorm(x, norm2_g[l]) * (1.0 + sc2) + sh2
        x = x + gt2 * moe_ffn(hn, router_w[l], router_b[l], exp_w1[l], exp_b1[l], exp_w2[l], exp_b2[l])
    return rmsnorm(x, final_norm_g)
```

```python
import numpy as np
import os
from contextlib import ExitStack
import concourse.bass as bass
import concourse.mybir as mybir
from concourse.bass_utils import run_bass_kernel_spmd

F32 = mybir.dt.float32
I32 = mybir.dt.int32
U32 = mybir.dt.uint32
AF = mybir.ActivationFunctionType
ALU = mybir.AluOpType
AX = mybir.AxisListType

D = 1024
N_IN = 4104
NE = 32
DEPTH = 2
EB = 512

ENG_NAMES = ['pe', 'act', 'dve', 'pool', 'sp']
SAME_ENGINE_SYNC = {'pe': False, 'act': True, 'dve': True, 'pool': True, 'sp': False}
EPOCH = 20000


class Prog:
    def __init__(self, nc, stack, n_dma_sems=12):
        self.nc = nc
        self.stack = stack
        self.q = {e: [] for e in ENG_NAMES}
        self.semobjs = {}
        self.epoch = {e: 0 for e in ENG_NAMES}
        self.cnt = {e: 0 for e in ENG_NAMES}
        self.cursem = {}
        for e in ENG_NAMES:
            self._new_epoch(e, first=True)
        self.waited = {e: {} for e in ENG_NAMES}
        self.dsems = {}
        for e in ['sp', 'act', 'pool']:
            self.dsems[e] = []
            for i in range(n_dma_sems):
                nm = "d_%s_%d" % (e, i)
                s = stack.enter_context(nc.semaphore(nm))
                self.semobjs[nm] = s
                self.dsems[e].append([nm, 0])
        self.drr = {e: 0 for e in self.dsems}
        self.last_w = {}
        self.readers = {}
        self.n_inst = 0

    def _new_epoch(self, e, first=False):
        if not first:
            self.epoch[e] += 1
        nm = "s_%s_%d" % (e, self.epoch[e])
        self.semobjs[nm] = self.stack.enter_context(self.nc.semaphore(nm))
        self.cursem[e] = nm
        self.cnt[e] = 0

    def _wait(self, eng, tok):
        if tok is None:
            return
        sname, val, teng = tok
        if teng == eng and not SAME_ENGINE_SYNC[eng]:
            return
        if self.waited[eng].get(sname, 0) >= val:
            return
        self.waited[eng][sname] = val
        s = self.semobjs[sname]
        self.q[eng].append(lambda e, s=s, val=val: e.wait_ge(s, val))

    def _deps(self, eng, reads, writes):
        for k in reads:
            self._wait(eng, self.last_w.get(k))
        for k in writes:
            self._wait(eng, self.last_w.get(k))
            for t in self.readers.get(k, ()):
                self._wait(eng, t)

    def _commit(self, tok, reads, writes):
        for k in reads:
            self.readers.setdefault(k, []).append(tok)
        for k in writes:
            self.last_w[k] = tok
            self.readers[k] = []

    def op(self, eng, fn, reads=(), writes=()):
        self._deps(eng, reads, writes)
        if self.cnt[eng] >= EPOCH:
            self._new_epoch(eng)
        self.cnt[eng] += 1
        val = self.cnt[eng]
        nm = self.cursem[eng]
        s = self.semobjs[nm]
        self.q[eng].append(lambda e, fn=fn, s=s: fn(e).then_inc(s, 1))
        self._commit((nm, val, eng), reads, writes)
        self.n_inst += 1

    def dma(self, eng, fn, reads=(), writes=()):
        self._deps(eng, reads, writes)
        i = self.drr[eng]
        self.drr[eng] = (i + 1) % len(self.dsems[eng])
        ent = self.dsems[eng][i]
        if ent[1] > 0:
            self._wait(eng, (ent[0], ent[1], None))
        ent[1] += 16
        s = self.semobjs[ent[0]]
        self.q[eng].append(lambda e, fn=fn, s=s: fn(e).then_inc(s, 16))
        self._commit((ent[0], ent[1], None), reads, writes)
        self.n_inst += 1

    def fence(self, eng):
        if self.cnt[eng] == 0:
            return
        nm = self.cursem[eng]
        val = self.cnt[eng]
        if self.waited[eng].get(nm, 0) >= val:
            return
        self.waited[eng][nm] = val
        s = self.semobjs[nm]
        self.q[eng].append(lambda e, s=s, val=val: e.wait_ge(s, val))

    def barrier(self):
        toks = []
        for e in ENG_NAMES:
            if self.cnt[e] > 0:
                toks.append((self.cursem[e], self.cnt[e], e))
        for e in self.dsems:
            for nm, v in self.dsems[e]:
                if v > 0:
                    toks.append((nm, v, None))
        for e in ENG_NAMES:
            for t in toks:
                if t[2] != e:
                    self._wait(e, t)
        self.last_w = {}
        self.readers = {}

    def emit(self):
        with self.nc.Block() as block:
            @block.sync
            def _(e):
                for f in self.q['sp']:
                    f(e)

            @block.scalar
            def _(e):
                for f in self.q['act']:
                    f(e)

            @block.vector
            def _(e):
                for f in self.q['dve']:
                    f(e)

            @block.gpsimd
            def _(e):
                for f in self.q['pool']:
                    f(e)

            @block.tensor
            def _(e):
                for f in self.q['pe']:
                    f(e)


def make_consts(T, pos0, n_blk):
    c = {}
    c['ident'] = np.eye(128, dtype=np.float32)
    i = np.arange(128)
    same = (i[:, None] // 64) == (i[None, :] // 64)
    le = i[:, None] <= i[None, :]
    lt = i[:, None] < i[None, :]
    c['m_incl_T'] = (same & le).astype(np.float32)
    c['m_incl_T_u'] = (same & le).astype(np.uint32)
    c['m_strict'] = (same & lt.T).astype(np.float32)
    c['m_same'] = same.astype(np.float32)
    c['tri_strict'] = lt.astype(np.float32)
    c['ones'] = np.ones((128, 128), np.float32)
    blk = np.zeros((128, 128), np.float32)
    blk[:64, :64] = 1
    blk[64:, 64:] = 1
    c['ones_bd'] = blk
    SEG = 512
    rm = np.ones((128, SEG), np.float32)
    rm[:, ::64] = 0
    c['resetmask'] = rm
    ca = np.zeros((128, SEG), np.float32)
    cb = np.zeros((128, SEG), np.float32)
    for ch in range(SEG // 64):
        (ca if ch % 2 == 0 else cb)[:, ch * 64:(ch + 1) * 64] = 1
    c['colA'] = ca
    c['colB'] = cb
    tt_ = np.arange(SEG) % 64
    c['mH0'] = np.broadcast_to((tt_ < 32).astype(np.float32), (128, SEG)).copy()
    c['mH1'] = np.broadcast_to((tt_ >= 32).astype(np.float32), (128, SEG)).copy()
    H = 4
    log_g = np.log1p(-np.exp2(-5.0 - np.arange(H, dtype=np.float64)))
    dt = np.zeros((128, H, 128), np.float64)
    for h in range(H):
        dd = (i[None, :] - i[:, None]).astype(np.float64)
        dt[:, h, :] = np.where(same & le, np.exp(dd * log_g[h]) * 0.125, 0.0)
    c['ret_DT'] = dt.astype(np.float32)
    tmod = i % 64
    qw = np.zeros((128, 2, 128), np.float64)
    for h in range(H):
        hp = (h % 2) * 64
        qw[hp:hp + 64, h // 2, :] = np.exp(log_g[h] * (tmod + 1.0))[None, :]
    qa = qw.copy()
    qa[:, :, 64:] = 0
    qb = qw.copy()
    qb[:, :, :64] = 0
    c['ret_qwA'] = qa.astype(np.float32)
    c['ret_qwB'] = qb.astype(np.float32)
    kw = np.zeros((128, H), np.float64)
    for h in range(H):
        kw[:, h] = np.exp(log_g[h] * (63.0 - tmod)) * 0.125
    c['ret_kw'] = kw.astype(np.float32)
    g64 = np.zeros((128, 2), np.float64)
    for h in range(H):
        hp = (h % 2) * 64
        g64[hp:hp + 64, h // 2] = np.exp(64.0 * log_g[h])
    c['ret_g64'] = g64.astype(np.float32)
    half = 32
    inv = (10000.0 ** (-np.linspace(0.0, 1.0, half, dtype=np.float32))).astype(np.float32)
    ang = (np.arange(pos0, pos0 + T, dtype=np.float32)[:, None] * inv[None, :]).astype(np.float32)
    c['cos'] = np.cos(ang.astype(np.float64)).astype(np.float32)
    c['sin'] = np.sin(ang.astype(np.float64)).astype(np.float32)
    jg = np.zeros((128, n_blk, NE), np.float32)
    jg[:] = (np.arange(n_blk, dtype=np.float32) * EB)[None, :, None]
    c['jgrid'] = jg
    ib = np.zeros((128, 8), np.float32)
    ib[:] = np.arange(8)[None, :] * 128 + np.arange(128)[:, None]
    c['idxbase'] = ib
    return c


CONST_SHAPES = None


class KB:
    def __init__(self, T, dbg=False, stop_after=None, depth=DEPTH, small_w=False):
        self.small_w = small_w
        self.T = T
        self.dbg = dbg
        self.nT = T // 128
        self.NK = T * 4
        self.n_blk = self.NK // EB + NE
        self.PS = self.n_blk * EB
        self.stop_after = stop_after
        self.depth = depth
        self.nc = bass.Bass("TRN2", target_bir_lowering=False)
        self.build()

    def din(self, name, shape, dt=F32):
        return self.nc.dram_tensor(name, list(shape), dt, kind="ExternalInput").ap()

    def dscr(self, name, shape, dt=F32, out=False):
        kind = "ExternalOutput" if (out or self.dbg) else "Internal"
        return self.nc.dram_tensor(name, list(shape), dt, kind=kind).ap()

    def alloc(self, name, cols, dt=F32):
        off = self.aoff
        self.aoff += cols
        assert self.aoff <= self.ACOLS, (name, self.aoff)
        ap = self.arena[:, off:off + cols]
        if dt != F32:
            ap = ap.bitcast(dt)
        return ap

    def mm(self, out, lhsT, rhs, start, stop, R, W, fence=False):
        if fence:
            self.P.fence('pe')
        self.P.op('pe', lambda e: e.matmul(out, lhsT=lhsT, rhs=rhs, start=start, stop=stop), reads=R, writes=W)

    def tr(self, out, in_, R, W):
        ident = self.ident[0:in_.shape[0], 0:in_.shape[0]]
        self.P.op('pe', lambda e: e.transpose(out=out, in_=in_, identity=ident), reads=list(R) + ['const'], writes=W)

    def act(self, out, in_, func, R, W, bias=None, scale=None, accum=None, eng='act'):
        kw = {}
        if bias is not None:
            kw['bias'] = bias
        if scale is not None:
            kw['scale'] = scale
        if accum is not None:
            kw['accum_out'] = accum
        self.P.op('act', lambda e: e.activation(out=out, in_=in_, func=func, **kw), reads=R, writes=W)

    def tt(self, eng, out, in0, in1, op, R, W):
        self.P.op(eng, lambda e: e.tensor_tensor(out=out, in0=in0, in1=in1, op=op), reads=R, writes=W)

    def ts(self, eng, out, in0, s1, s2, op0, op1, R, W):
        if op1 is None:
            self.P.op(eng, lambda e: e.tensor_scalar(out=out, in0=in0, scalar1=s1, scalar2=None, op0=op0), reads=R, writes=W)
        else:
            self.P.op(eng, lambda e: e.tensor_scalar(out=out, in0=in0, scalar1=s1, scalar2=s2, op0=op0, op1=op1), reads=R, writes=W)

    def stt(self, out, in0, scalar, in1, op0, op1, R, W, eng='dve'):
        self.P.op(eng, lambda e: e.scalar_tensor_tensor(out=out, in0=in0, scalar=scalar, in1=in1, op0=op0, op1=op1), reads=R, writes=W)

    def cp(self, eng, out, in_, R, W):
        if eng == 'act':
            self.P.op('act', lambda e: e.copy(out=out, in_=in_), reads=R, writes=W)
        else:
            self.P.op(eng, lambda e: e.tensor_copy(out=out, in_=in_), reads=R, writes=W)

    def memset(self, eng, out, val, W):
        self.P.op(eng, lambda e: e.memset(out, val), reads=(), writes=W)

    def dma(self, eng, out, in_, R, W):
        self.P.dma(eng, lambda e: e.dma_start(out=out, in_=in_), reads=R, writes=W)

    def dma_nc(self, eng, out, in_, R, W):
        self.P.dma(eng, lambda e: e.dma_start(out=out, in_=in_, allow_slow_non_contiguous=True), reads=R, writes=W)

    def gather(self, out, src, idx, R, W):
        self.P.dma('pool', lambda e: e.indirect_dma_start(out=out, out_offset=None, in_=src,
                                                         in_offset=bass.IndirectOffsetOnAxis(ap=idx, axis=0)),
                   reads=R, writes=W)

    def scatter(self, dst, src, idx, R, W):
        self.P.dma('pool', lambda e: e.indirect_dma_start(out=dst, out_offset=bass.IndirectOffsetOnAxis(ap=idx, axis=0),
                                                         in_=src, in_offset=None),
                   reads=R, writes=W)

    def rstd_from_ss(self, ss, n, R):
        pass

    def build(self):
        nc = self.nc
        T, nT = self.T, self.nT
        self.x_in = self.din("x", [T, D])
        self.cT = self.din("cT", [128, 8])
        self.ada_w = self.din("ada_w", [DEPTH, D, 6 * D])
        self.ada_b = self.din("ada_b", [DEPTH, 6 * D])
        self.norm1_g = self.din("norm1_g", [DEPTH, D])
        self.norm2_g = self.din("norm2_g", [DEPTH, D])
        self.w_in = self.din("w_in", [DEPTH, D, N_IN])
        self.w_out = self.din("w_out", [DEPTH, D, D])
        self.hg_lb = self.din("hg_lb_logits", [DEPTH, 512])
        self.mixg = self.din("mixg", [DEPTH, D])
        self.conv_w = self.din("gdn_conv_w", [DEPTH, 4, 768])
        self.A_log = self.din("gdn_A_log", [DEPTH, 4])
        self.dt_bias = self.din("gdn_dt_bias", [DEPTH, 4])
        self.router_w = self.din("router_w", [DEPTH, D, NE])
        self.router_b = self.din("router_b", [DEPTH, NE])
        nrow = 128 if self.small_w else DEPTH * NE * D
        self.w1 = self.din("exp_w1", [nrow, 2 * D])
        self.b1 = self.din("exp_b1", [DEPTH * NE, 2 * D])
        self.w2 = self.din("exp_w2", [nrow, D])
        self.b2 = self.din("exp_b2", [DEPTH * NE, D])
        self.fin_g = self.din("final_norm_g", [D])
        cs = make_consts(128, 0, self.n_blk)
        self.cd = {}
        for k, v in cs.items():
            shp = list(v.shape)
            if k in ('cos', 'sin'):
                shp = [T, 32]
            self.cd[k] = self.din("c_" + k, shp, U32 if v.dtype == np.uint32 else F32)
        self.mod_d = self.dscr("mod_d", [DEPTH, 6 * D])
        self.zT_d = self.dscr("zT_d", [1800, T])
        self.zK_d = self.dscr("zK_d", [T, 2312])
        self.o_d = self.dscr("o_d", [T, D])
        self.gcrow_d = self.dscr("gcrow_d", [4, T])
        self.xa_d = self.dscr("xa_d", [T, D])
        self.xb_d = self.dscr("xb_d", [T, D])
        self.hn_d = self.dscr("hn_d", [T, D])
        self.xg_d = self.dscr("xg_d", [self.PS, D])
        self.yg_d = self.dscr("yg_d", [self.PS, D])
        self.out_d = self.nc.dram_tensor("out", [T, D], F32, kind="ExternalOutput").ap()

        with ExitStack() as st:
            self.P = Prog(nc, st)
            self.ACOLS = 48000
            self.arena = st.enter_context(nc.sbuf_tensor("arena", [128, self.ACOLS], F32))
            self.pb = [st.enter_context(nc.psum_tensor("pb%d" % i, [128, 512], F32)) for i in range(8)]
            self.aoff = 0
            self.ident = self.alloc("ident", 128)
            self.dma('sp', self.ident, self.cd['ident'], [], ['const'])
            self.amark = self.aoff
            self.P.barrier()
            self.prologue()
            xin = self.x_in
            bufs = [self.xa_d, self.xb_d]
            for l in range(self.depth):
                self.m1(l, xin)
                if self.stop_after == ('m1', l):
                    break
                self.m2(l)
                self.m3(l)
                self.m4(l)
                if self.stop_after == ('m4', l):
                    break
                self.m5(l, xin, bufs[0])
                if self.stop_after == ('m5', l):
                    break
                self.moe(l, bufs[0], bufs[1], last=(l == self.depth - 1))
                if self.stop_after == ('moe', l):
                    break
                xin = bufs[1]
            self.P.barrier()
            self.P.emit()
            self.n_inst = self.P.n_inst

    def phase_end(self):
        self.P.barrier()
        self.aoff = self.amark

    def prologue(self):
        condT = self.alloc("condT", 8)
        self.dma('sp', condT, self.cT, [], ['condT'])
        self.act(condT, condT, AF.Silu, ['condT'], ['condT'])
        row = self.alloc("modrow", 6 * D)
        brow = self.alloc("brow", 6 * D)
        aw = [self.alloc("aw0", 8 * 512), self.alloc("aw1", 8 * 512)]
        for l in range(DEPTH):
            self.dma('act', brow[0:1, :], self.ada_b[l:l + 1, :], [], ['brow'])
            for cgi in range(12):
                b = aw[cgi % 2]
                bk = 'aw%d' % (cgi % 2)
                b3 = b.rearrange("p (k n) -> p k n", n=512)
                self.dma('sp', b3, self.ada_w[l][:, cgi * 512:(cgi + 1) * 512].rearrange("(k p) n -> p k n", p=128), [], [bk])
                ps = self.pb[cgi % 2]
                for k in range(8):
                    self.mm(ps[0:1, :], condT[:, k:k + 1], b3[:, k, :], k == 0, k == 7, ['condT', bk], ['pb%d' % (cgi % 2)])
                self.tt('dve', row[0:1, cgi * 512:(cgi + 1) * 512], ps[0:1, :], brow[0:1, cgi * 512:(cgi + 1) * 512], ALU.add,
                        ['brow'], ['modrow', 'pb%d' % (cgi % 2)])
            self.dma('sp', self.mod_d[l:l + 1, :], row[0:1, :], ['modrow'], ['mod_d'])
        self.phase_end()

    def load_mod(self, l, which, gsrc):
        G = self.alloc("G", D)
        S = self.alloc("S", D)
        tmp = self.alloc("Gtmp", D)
        base = 0 if which == 1 else 3 * D
        self.dma('sp', G, gsrc.partition_broadcast(128), [], ['G'])
        self.dma('act', tmp, self.mod_d[l, base + D:base + 2 * D].partition_broadcast(128), [], ['Gtmp'])
        self.dma('sp', S, self.mod_d[l, base:base + D].partition_broadcast(128), [], ['S'])
        self.stt(G, tmp, 1.0, G, ALU.add, ALU.mult, ['G', 'Gtmp'], ['G'])
        return G, S

    def norm_tile(self, src_rows, G, S, xt, hn, ss, tag, eps=1e-6):
        self.dma('sp', xt, src_rows, [], [tag + 'x'])
        self.act(hn, xt, AF.Square, [tag + 'x'], [tag + 'hn', tag + 'ss'], accum=ss)
        self.ts('dve', ss, ss, 1.0 / D, eps, ALU.mult, ALU.add, [tag + 'ss'], [tag + 'ss'])
        self.act(ss, ss, AF.Sqrt, [tag + 'ss'], [tag + 'ss'])
        self.P.op('dve', lambda e: e.reciprocal(out=ss, in_=ss), reads=[tag + 'ss'], writes=[tag + 'ss'])
        self.stt(hn, xt, ss, G, ALU.mult, ALU.mult, [tag + 'x', tag + 'ss', 'G', tag + 'hn'], [tag + 'hn'])
        if S is not None:
            self.tt('pool', hn, hn, S, ALU.add, [tag + 'hn', 'S'], [tag + 'hn'])

    def transpose_tile(self, hn, hnT3, col0, tag, tkey, evac=('act', 'dve')):
        for half in range(2):
            ps = self.pb[6 + half]
            pk = 'pb%d' % (6 + half)
            ps3 = ps.rearrange("p (j n) -> p j n", n=128)
            for j in range(4):
                k = half * 4 + j
                self.tr(ps3[:, j, :], hn[:, k * 128:(k + 1) * 128], [tag + 'hn'], [pk])
            self.cp(evac[half], hnT3[:, half * 4:(half + 1) * 4, col0:col0 + 128], ps3, [], [tkey, pk])

    def m1(self, l, xin):
        T, nT = self.T, self.nT
        G, S = self.load_mod(l, 1, self.norm1_g[l])
        win = self.w_in[l].rearrange("(k p) n -> p k n", p=128)
        mark = self.aoff
        WF = self.alloc("WF", 8 * 1800).rearrange("p (k n) -> p k n", n=1800)
        self.dma('sp', WF[:, :, 0:1024], win[:, :, 0:1024], [], ['WF'])
        self.dma('act', WF[:, :, 1024:1792], win[:, :, 3072:3840], [], ['WF'])
        self.dma('sp', WF[:, :, 1792:1800], win[:, :, 4096:4104], [], ['WF'])
        NB = 512 if T >= 512 else T
        xt = [self.alloc("xt0", D), self.alloc("xt1", D)]
        hn = [self.alloc("hn0", D), self.alloc("hn1", D)]
        ss = [self.alloc("ss0", 1), self.alloc("ss1", 1)]
        hnT = [self.alloc("hnTa", 8 * NB).rearrange("p (k n) -> p k n", n=NB),
               self.alloc("hnTb", 8 * NB).rearrange("p (k n) -> p k n", n=NB)]
        stg = [self.alloc("stg%d" % i, NB) for i in range(4)]
        npb = NB // 128
        nblk = T // NB

        def normF(blk, j):
            it_ = blk * npb + j
            self.norm_tile(xin[it_ * 128:(it_ + 1) * 128, :], G, S, xt[it_ % 2], hn[it_ % 2], ss[it_ % 2], 't%d' % (it_ % 2))

        def trF(blk, j):
            it_ = blk * npb + j
            self.transpose_tile(hn[it_ % 2], hnT[blk % 2], j * 128, 't%d' % (it_ % 2), 'hnT%d' % (blk % 2))

        for j in range(npb):
            normF(0, j)
            trF(0, j)
        nslots = {3 * j: j for j in range(npb)}
        tslots = {3 * j + 2: j for j in range(npb)}
        for blk in range(nblk):
            hT = hnT[blk % 2]
            hk = 'hnT%d' % (blk % 2)
            for mt in range(15):
                msz = 128 if mt < 14 else 8
                ps = self.pb[mt % 4]
                pk = 'pb%d' % (mt % 4)
                for k in range(8):
                    self.mm(ps[0:msz, 0:NB], WF[:, k, mt * 128:mt * 128 + msz], hT[:, k, :], k == 0, k == 7, ['WF', hk], [pk])
                sg = stg[mt % 4]
                sk = 'stg%d' % (mt % 4)
                self.cp('act' if mt % 2 == 0 else 'dve', sg[0:msz, :], ps[0:msz, 0:NB], [], [sk, pk])
                self.dma('sp' if mt % 2 == 0 else 'act', self.zT_d[mt * 128:mt * 128 + msz, blk * NB:(blk + 1) * NB], sg[0:msz, :], [sk], ['zT_d'])
                if blk + 1 < nblk and mt in nslots:
                    normF(blk + 1, nslots[mt])
                if blk + 1 < nblk and mt in tslots:
                    trF(blk + 1, tslots[mt])
        self.P.barrier()
        self.aoff = mark
        WK = self.alloc("WK", 8 * 2312).rearrange("p (k n) -> p k n", n=2312)
        self.dma('sp', WK[:, :, 0:1024], win[:, :, 1024:2048], [], ['WK'])
        self.dma('act', WK[:, :, 1024:2048], win[:, :, 2048:3072], [], ['WK'])
        self.dma('sp', WK[:, :, 2048:2312], win[:, :, 3840:4104], [], ['WK'])
        xt = [self.alloc("xt0", D), self.alloc("xt1", D)]
        hn = [self.alloc("hn0", D), self.alloc("hn1", D)]
        ss = [self.alloc("ss0", 1), self.alloc("ss1", 1)]
        hnT = [self.alloc("hnTa", 8 * 128).rearrange("p (k n) -> p k n", n=128),
               self.alloc("hnTb", 8 * 128).rearrange("p (k n) -> p k n", n=128)]
        zst = [self.alloc("zst0", 2312), self.alloc("zst1", 2312)]
        chunks = [(0, 512), (512, 1024), (1024, 1536), (1536, 2048), (2048, 2312)]
        def normK(i):
            self.norm_tile(xin[i * 128:(i + 1) * 128, :], G, S, xt[i % 2], hn[i % 2], ss[i % 2], 't%d' % (i % 2))

        def trK(i):
            self.transpose_tile(hn[i % 2], hnT[i % 2], 0, 't%d' % (i % 2), 'hnT%d' % (i % 2))

        normK(0)
        trK(0)
        for i in range(nT):
            hk = 'hnT%d' % (i % 2)
            for ci, (c0, c1) in enumerate(chunks):
                ps = self.pb[ci % 4]
                pk = 'pb%d' % (ci % 4)
                for k in range(8):
                    self.mm(ps[:, 0:c1 - c0], hnT[i % 2][:, k, :], WK[:, k, c0:c1], k == 0, k == 7, [hk, 'WK'], [pk])
                self.cp('act' if ci % 2 == 0 else 'dve', zst[i % 2][:, c0:c1], ps[:, 0:c1 - c0], [], ['zst%d' % (i % 2), pk])
                if ci == 0 and i + 1 < nT:
                    normK(i + 1)
                if ci == 3 and i + 1 < nT:
                    trK(i + 1)
            self.dma('sp', self.zK_d[i * 128:(i + 1) * 128, :], zst[i % 2], ['zst%d' % (i % 2)], ['zK_d'])
        self.phase_end()


def prep_inputs(inp, b, T, pos0, n_blk, s0=0, small_w=False):
    m = {}
    m["x"] = np.ascontiguousarray(inp["x"][b, s0:s0 + T])
    m["cT"] = np.ascontiguousarray(inp["c"][b].reshape(8, 128).T)
    for k in ["ada_w", "ada_b", "norm1_g", "norm2_g", "w_in", "w_out", "gdn_conv_w", "gdn_A_log", "gdn_dt_bias",
              "router_w", "router_b", "final_norm_g"]:
        m[k] = np.ascontiguousarray(inp[k])
    m["hg_lb_logits"] = np.ascontiguousarray(inp["hg_lb_logits"])
    m["mixg"] = np.ascontiguousarray(np.concatenate([inp["hg_norm_g"].reshape(DEPTH, 512), inp["ret_norm_g"].reshape(DEPTH, 256),
                                                      inp["gdn_norm_g"].reshape(DEPTH, 256)], axis=1))
    perm = np.concatenate([np.arange(0, 512), np.arange(1024, 1536), np.arange(512, 1024), np.arange(1536, 2048)])
    m["exp_w1"] = np.ascontiguousarray(inp["exp_w1"].reshape(DEPTH * NE * D, 2 * D)[:, perm])
    m["exp_b1"] = np.ascontiguousarray(inp["exp_b1"].reshape(DEPTH * NE, 2 * D)[:, perm])
    m["exp_w2"] = np.ascontiguousarray(inp["exp_w2"]).reshape(DEPTH * NE * D, D)
    m["exp_b2"] = np.ascontiguousarray(inp["exp_b2"]).reshape(DEPTH * NE, D)
    if small_w:
        m["exp_w1"] = m["exp_w1"][:128]
        m["exp_w2"] = m["exp_w2"][:128]
    cs = make_consts(T, pos0, n_blk)
    for k, v in cs.items():
        m["c_" + k] = v
    return m


def _m2(self, l):
    T = self.T
    SEG = min(512, T)
    nseg = T // SEG
    tps = SEG // 128
    nch = SEG // 64
    A = self.alloc
    rmask = A("rmask", SEG); colA = A("colA", SEG); colB = A("colB", SEG)
    mF = A("mF", 128)
    mH0 = A("mH0", SEG); mH1 = A("mH1", SEG)
    self.dma('sp', rmask, self.cd['resetmask'][:, 0:SEG], [], ['const2'])
    self.dma('sp', colA, self.cd['colA'][:, 0:SEG], [], ['const2'])
    self.dma('act', colB, self.cd['colB'][:, 0:SEG], [], ['const2'])
    self.dma('act', mF, self.cd['m_incl_T'], [], ['const2'])
    self.dma('sp', mH0, self.cd['mH0'][:, 0:SEG], [], ['const2'])
    self.dma('act', mH1, self.cd['mH1'][:, 0:SEG], [], ['const2'])
    lbv = A("lbv", 4); omlb = A("omlb", 4); lx0 = A("lx0", 4)
    if l == 0:
        self.memset('dve', lbv, 0.0, ['lb'])
        self.memset('dve', omlb, 1.0, ['lb'])
    else:
        self.dma_nc('sp', lx0, self.hg_lb[0].rearrange("(h d) -> d h", d=128), [], ['lx0'])
        self.dma_nc('sp', lbv, self.hg_lb[1].rearrange("(h d) -> d h", d=128), [], ['lb'])
        self.tt('dve', lbv, lbv, lx0, ALU.subtract, ['lb', 'lx0'], ['lb'])
        self.act(lbv, lbv, AF.Sigmoid, ['lb'], ['lb'])
        self.ts('dve', omlb, lbv, -1.0, 1.0, ALU.mult, ALU.add, ['lb'], ['lb'])
    Sst = A("Sst", 512).rearrange("p (h e) -> p h e", e=128)
    self.memset('pool', Sst, 0.0, ['S0', 'S1', 'S2', 'S3'])
    sTs = [A("sT%d" % i, 128) for i in range(4)]
    for i in range(4):
        self.memset('pool', sTs[i], 0.0, ['sT%d' % i])
    Vt = A("Vt", tps * 512).rearrange("p (t n) -> p t n", n=512)
    ost = [A("ost%d" % i, 128) for i in range(4)]
    names = ['q', 'f', 'lf', 'k', 'b', 'd', 'e', 'e3', 'Qi', 'Ki', 'QdA', 'QdB', 'Kd', 'Qd', 'QdX', 'Ki0']
    sets = []
    for h in range(4):
        s = {n: A("%s%d" % (n, h), SEG) for n in names}
        s['KdTok'] = A("KdTok%d" % h, tps * 128).rearrange("p (t n) -> p t n", n=128)
        sets.append(s)
    for sg in range(nseg):
        c0 = sg * SEG
        self.dma('sp', Vt, self.zK_d[c0:c0 + SEG, 0:512].rearrange("(t p) n -> p t n", p=128), [], ['Vt'])
        for h in range(4):
            s = sets[h]
            K = lambda n: "%s%d" % (n, h)
            self.dma('sp', s['q'], self.zT_d[h * 128:(h + 1) * 128, c0:c0 + SEG], [], [K('q')])
            self.dma('act', s['f'], self.zT_d[512 + h * 128:512 + (h + 1) * 128, c0:c0 + SEG], [], [K('f')])
            self.act(s['q'], s['q'], AF.Silu, [K('q')], [K('q')])
            self.act(s['f'], s['f'], AF.Sigmoid, [K('f')], [K('f')])
            self.ts('dve', s['f'], s['f'], omlb[:, h:h + 1], lbv[:, h:h + 1], ALU.mult, ALU.add, [K('f'), 'lb'], [K('f')])
            self.act(s['lf'], s['f'], AF.Ln, [K('f')], [K('lf')])
            self.ts('pool', s['k'], s['f'], -1.0, 1.0, ALU.mult, ALU.add, [K('f')], [K('k')])
            self.P.op('dve', lambda e, s=s: e.tensor_tensor_scan(out=s['b'], data0=rmask, data1=s['lf'], initial=0.0, op0=ALU.mult, op1=ALU.add),
                      reads=[K('lf'), 'const2'], writes=[K('b')])
            b3 = s['b'].rearrange("p (c t) -> p c t", t=64)
            d3 = s['d'].rearrange("p (c t) -> p c t", t=64)
            self.tt('dve', d3, b3, b3[:, :, 31:32].to_broadcast([128, nch, 64]), ALU.subtract, [K('b')], [K('d')])
            self.ts('dve', s['d'], s['d'], -80.0, 80.0, ALU.max, ALU.min, [K('d')], [K('d')])
            self.act(s['e'], s['d'], AF.Exp, [K('d')], [K('e')])
            self.tt('dve', s['Qi'], s['q'], s['e'], ALU.mult, [K('q'), K('e')], [K('Qi')])
            self.tt('pool', s['Qi'], s['Qi'], mH1, ALU.mult, [K('Qi'), 'const2'], [K('Qi')])
            self.act(s['e'], s['d'], AF.Exp, [K('d')], [K('e')], scale=-1.0)
            self.tt('pool', s['Ki'], s['k'], s['e'], ALU.mult, [K('k'), K('e')], [K('Ki')])
            self.act(s['e3'], s['b'], AF.Exp, [K('b')], [K('e3')])
            self.tt('pool', s['Qd'], s['q'], s['e3'], ALU.mult, [K('q'), K('e3')], [K('Qd')])
            self.tt('pool', s['QdA'], s['Qd'], colA, ALU.mult, [K('Qd'), 'const2'], [K('QdA')])
            self.tt('pool', s['QdB'], s['Qd'], colB, ALU.mult, [K('Qd'), 'const2'], [K('QdB')])
            self.tt('pool', s['QdX'], s['Qd'], mH0, ALU.mult, [K('Qd'), 'const2'], [K('QdX')])
            self.tt('dve', d3, b3, b3[:, :, 63:64].to_broadcast([128, nch, 64]), ALU.subtract, [K('b')], [K('d')])
            self.act(s['e'], s['d'], AF.Exp, [K('d')], [K('e')], scale=-1.0)
            self.tt('dve', s['Kd'], s['k'], s['e'], ALU.mult, [K('k'), K('e')], [K('Kd')])
            self.ts('dve', s['d'], s['b'], -1.0, 80.0, ALU.mult, ALU.min, [K('b')], [K('d')])
            self.act(s['e'], s['d'], AF.Exp, [K('d')], [K('e')])
            self.tt('dve', s['Ki0'], s['k'], s['e'], ALU.mult, [K('k'), K('e')], [K('Ki0')])
            self.tt('pool', s['Ki0'], s['Ki0'], mH0, ALU.mult, [K('Ki0'), 'const2'], [K('Ki0')])
        for ti in range(tps):
            tk = slice(ti * 128, (ti + 1) * 128)
            row0 = c0 + ti * 128
            BA = lambda h: self.pb[2 * h]
            BB = lambda h: self.pb[2 * h + 1]
            KA = lambda h: 'pb%d' % (2 * h)
            KBk = lambda h: 'pb%d' % (2 * h + 1)
            if int(os.environ.get('M2LVL', '9')) < 1:
                continue
            for h in range(4):
                s = sets[h]
                K = lambda n: "%s%d" % (n, h)
                self.mm(BA(h)[:, 0:128], s['Ki'][:, tk], s['Qi'][:, tk], True, False, [K('Ki'), K('Qi')], [KA(h)])
                self.mm(BA(h)[:, 0:128], s['Ki0'][:, tk], s['QdX'][:, tk], False, True, [K('Ki0'), K('QdX')], [KA(h)])
                self.tr(BA(h)[:, 128:256], s['Kd'][:, tk], [K('Kd')], [KA(h)])
            if int(os.environ.get('M2LVL', '9')) < 2:
                continue
            for h in range(4):
                s = sets[h]
                K = lambda n: "%s%d" % (n, h)
                self.tt('dve', sTs[h], BA(h)[:, 0:128], mF, ALU.mult, ['const2'], ['sT%d' % h, KA(h)])
                self.cp('act', s['KdTok'][:, ti, :], BA(h)[:, 128:256], [], [K('KdTok'), KA(h)])
            if int(os.environ.get('M2LVL', '9')) < 3:
                continue
            for h in range(4):
                s = sets[h]
                K = lambda n: "%s%d" % (n, h)
                vh = Vt[:, ti, h * 128:(h + 1) * 128]
                SUB = os.environ.get('M2SUB', '1234')
                if '1' in SUB:
                    self.mm(BB(h)[:, 0:128], sTs[h], vh, True, False, ['sT%d' % h, 'Vt'], [KBk(h)])
                if '2' in SUB:
                    self.mm(BB(h)[:, 0:128], s['QdA'][:, tk], Sst[:, h, :], False, False, [K('QdA'), 'S%d' % h], [KBk(h)])
                if '3' in SUB:
                    self.mm(BA(h)[:, 256:384], s['KdTok'][0:64, ti, :], Vt[0:64, ti, h * 128:(h + 1) * 128], True, True, [K('KdTok'), 'Vt'], [KA(h)])
                if '4' in SUB:
                    self.mm(BA(h)[:, 384:512], s['KdTok'][64:128, ti, :], Vt[64:128, ti, h * 128:(h + 1) * 128], True, True, [K('KdTok'), 'Vt'], [KA(h)], fence=True)
            if int(os.environ.get('M2LVL', '9')) < 4:
                continue
            for h in range(4):
                s = sets[h]
                K = lambda n: "%s%d" % (n, h)
                cA = (ti * 2) * 64 + 63
                self.stt(Sst[:, h, :], Sst[:, h, :], s['e3'][:, cA:cA + 1], BA(h)[:, 256:384], ALU.mult, ALU.add, ['S%d' % h, K('e3')], ['S%d' % h, KA(h)])
            if int(os.environ.get('M2LVL', '9')) < 5:
                continue
            for h in range(4):
                s = sets[h]
                K = lambda n: "%s%d" % (n, h)
                self.mm(BB(h)[:, 0:128], s['QdB'][:, tk], Sst[:, h, :], False, True, [K('QdB'), 'S%d' % h], [KBk(h)])
            if int(os.environ.get('M2LVL', '9')) < 6:
                continue
            for h in range(4):
                s = sets[h]
                K = lambda n: "%s%d" % (n, h)
                cB = (ti * 2 + 1) * 64 + 63
                self.stt(Sst[:, h, :], Sst[:, h, :], s['e3'][:, cB:cB + 1], BA(h)[:, 384:512], ALU.mult, ALU.add, ['S%d' % h, K('e3')], ['S%d' % h, KA(h)])
                self.cp('act', ost[h], BB(h)[:, 0:128], [], ['ost%d' % h, KBk(h)])
                self.dma('sp' if h % 2 == 0 else 'act', self.o_d[row0:row0 + 128, h * 128:(h + 1) * 128], ost[h], ['ost%d' % h], ['o_d'])
    self.phase_end()


KB.m2 = _m2
KB.m3 = lambda self, l: None
KB.m4 = lambda self, l: None


def _m3(self, l):
    T = self.T
    SEG = min(512, T)
    nseg = T // SEG
    tps = SEG // 128
    A = self.alloc
    DT = A("DT", 512).rearrange("p (h t) -> p h t", t=128)
    qwA = A("qwA", 256).rearrange("p (j t) -> p j t", t=128)
    qwB = A("qwB", 256).rearrange("p (j t) -> p j t", t=128)
    kw = A("kw", 4)
    g64 = A("g64", 2)
    self.dma('sp', DT, self.cd['ret_DT'], [], ['c3'])
    self.dma('sp', qwA, self.cd['ret_qwA'], [], ['c3'])
    self.dma('act', qwB, self.cd['ret_qwB'], [], ['c3'])
    self.dma('act', kw, self.cd['ret_kw'], [], ['c3'])
    self.dma('act', g64, self.cd['ret_g64'], [], ['c3'])
    S2 = A("S2r", 128).rearrange("p (j e) -> p j e", e=64)
    self.memset('pool', S2, 0.0, ['Sr0', 'Sr1'])
    qk = A("qk", tps * 512).rearrange("p (t n) -> p t n", n=512)
    rv = A("rv", tps * 256).rearrange("p (t n) -> p t n", n=256)
    cs = A("cs", tps * 32).rearrange("p (t n) -> p t n", n=32)
    sn = A("sn", tps * 32).rearrange("p (t n) -> p t n", n=32)
    NB = 2
    rot = [A("rot%d" % i, 512) for i in range(NB)]
    ta = [A("ta%d" % i, 256) for i in range(NB)]
    tb = [A("tb%d" % i, 256) for i in range(NB)]
    tc_ = [A("tc%d" % i, 256) for i in range(NB)]
    td = [A("td%d" % i, 256) for i in range(NB)]
    Kwp = [A("Kwp%d" % i, 512) for i in range(NB)]
    for i in range(NB):
        self.memset('pool', Kwp[i], 0.0, ['Kwp%d' % i])
    qT = [A("qT%d" % i, 256).rearrange("p (j t) -> p j t", t=128) for i in range(NB)]
    kT = [A("kT%d" % i, 256).rearrange("p (j t) -> p j t", t=128) for i in range(NB)]
    QA = [A("QA%d" % i, 256).rearrange("p (j t) -> p j t", t=128) for i in range(NB)]
    QB = [A("QB%d" % i, 256).rearrange("p (j t) -> p j t", t=128) for i in range(NB)]
    sTs = [A("rsT%d" % i, 128) for i in range(4)]
    ost = [A("rost%d" % i, 256) for i in range(NB)]
    it = 0
    for sg in range(nseg):
        c0 = sg * SEG
        self.dma('sp', qk, self.zK_d[c0:c0 + SEG, 1024:1536].rearrange("(t p) n -> p t n", p=128), [], ['qk'])
        self.dma('act', rv, self.zK_d[c0:c0 + SEG, 1536:1792].rearrange("(t p) n -> p t n", p=128), [], ['rv'])
        self.dma('sp', cs, self.cd['cos'][c0:c0 + SEG, :].rearrange("(t p) n -> p t n", p=128), [], ['cs'])
        self.dma('act', sn, self.cd['sin'][c0:c0 + SEG, :].rearrange("(t p) n -> p t n", p=128), [], ['sn'])
        for ti in range(tps):
            b = it % NB
            it += 1
            bk = lambda n: "%s%d" % (n, b)
            row0 = c0 + ti * 128
            v4 = qk[:, ti, :].rearrange("p (g two r) -> p g two r", g=8, two=2)
            t1 = v4[:, :, 0, :]
            t2 = v4[:, :, 1, :]
            o4 = rot[b].rearrange("p (g two r) -> p g two r", g=8, two=2)
            cb = cs[:, ti, :].unsqueeze(1).to_broadcast([128, 8, 32])
            sb_ = sn[:, ti, :].unsqueeze(1).to_broadcast([128, 8, 32])
            a3 = lambda x: x.rearrange("p (g r) -> p g r", r=32)
            self.tt('dve', a3(ta[b]), t1, cb, ALU.mult, ['qk', 'cs'], [bk('ta')])
            self.tt('dve', a3(tb[b]), t2, sb_, ALU.mult, ['qk', 'sn'], [bk('tb')])
            self.tt('dve', o4[:, :, 0, :], a3(ta[b]), a3(tb[b]), ALU.subtract, [bk('ta'), bk('tb')], [bk('rot')])
            self.tt('pool', a3(tc_[b]), t1, sb_, ALU.mult, ['qk', 'sn'], [bk('tc')])
            self.tt('pool', a3(td[b]), t2, cb, ALU.mult, ['qk', 'cs'], [bk('td')])
            self.tt('pool', o4[:, :, 1, :], a3(tc_[b]), a3(td[b]), ALU.add, [bk('tc'), bk('td')], [bk('rot')])
            kp = rot[b][:, 256:512].rearrange("p (j i d) -> p j i d", j=2, i=2)
            Kw4 = Kwp[b].rearrange("p (j i c) -> p j i c", j=2, i=2)
            kw3 = kw.rearrange("p (j i) -> p j i", i=2)
            for i in range(2):
                self.tt('dve' if i == 0 else 'pool', Kw4[:, :, i, i * 64:(i + 1) * 64], kp[:, :, i, :],
                        kw3[:, :, i:i + 1].to_broadcast([128, 2, 64]), ALU.mult, [bk('rot'), 'c3'], [bk('Kwp')])
            p0 = self.pb[0].rearrange("p (r t) -> p r t", t=128)
            for r in range(4):
                self.tr(p0[:, r, :], rot[b][:, r * 128:(r + 1) * 128], [bk('rot')], ['pb0'])
            self.cp('act', qT[b], p0[:, 0:2, :], [], [bk('qT'), 'pb0'])
            self.cp('act', kT[b], p0[:, 2:4, :], [], [bk('kT'), 'pb0'])
            self.tt('dve', QA[b], p0[:, 0:2, :], qwA, ALU.mult, ['c3'], [bk('QA'), 'pb0'])
            self.tt('dve', QB[b], p0[:, 0:2, :], qwB, ALU.mult, ['c3'], [bk('QB'), 'pb0'])
            for h in range(4):
                j, hp = h // 2, (h % 2) * 64
                bank = 1 + (h % 2)
                self.mm(self.pb[bank][:, j * 128:(j + 1) * 128], kT[b][hp:hp + 64, j, :], qT[b][hp:hp + 64, j, :], True, True,
                        [bk('kT'), bk('qT')], ['pb%d' % bank])
            for h in range(4):
                j = h // 2
                bank = 1 + (h % 2)
                self.tt('dve', sTs[h], self.pb[bank][:, j * 128:(j + 1) * 128], DT[:, h, :], ALU.mult, ['c3'], ['rsT%d' % h, 'pb%d' % bank])
            for h in range(4):
                j, hp = h // 2, (h % 2) * 64
                ob = self.pb[3 + h][:, 0:64]
                self.mm(ob, sTs[h], rv[:, ti, h * 64:(h + 1) * 64], True, False, ['rsT%d' % h, 'rv'], ['pb%d' % (3 + h)])
                self.mm(ob, QA[b][hp:hp + 64, j, :], S2[hp:hp + 64, j, :], False, False, [bk('QA'), 'Sr%d' % j], ['pb%d' % (3 + h)])
            for j in range(2):
                kvb = self.pb[7][:, j * 64:(j + 1) * 64]
                for i in range(2):
                    h = 2 * j + i
                    self.mm(kvb, Kwp[b][0:64, h * 128:(h + 1) * 128], rv[0:64, ti, h * 64:(h + 1) * 64], i == 0, i == 1,
                            [bk('Kwp'), 'rv'], ['pb7'])
            for j in range(2):
                self.stt(S2[:, j, :], S2[:, j, :], g64[:, j:j + 1], self.pb[7][:, j * 64:(j + 1) * 64], ALU.mult, ALU.add,
                         ['Sr%d' % j, 'c3'], ['Sr%d' % j, 'pb7'])
            for h in range(4):
                j, hp = h // 2, (h % 2) * 64
                ob = self.pb[3 + h][:, 0:64]
                self.mm(ob, QB[b][hp:hp + 64, j, :], S2[hp:hp + 64, j, :], False, True, [bk('QB'), 'Sr%d' % j], ['pb%d' % (3 + h)])
            for j in range(2):
                kvb = self.pb[0][:, j * 64:(j + 1) * 64]
                for i in range(2):
                    h = 2 * j + i
                    self.mm(kvb, Kwp[b][64:128, h * 128:(h + 1) * 128], rv[64:128, ti, h * 64:(h + 1) * 64], i == 0, i == 1,
                            [bk('Kwp'), 'rv'], ['pb0'], fence=(i == 0 and j == 0))
            for j in range(2):
                self.stt(S2[:, j, :], S2[:, j, :], g64[:, j:j + 1], self.pb[0][:, j * 64:(j + 1) * 64], ALU.mult, ALU.add,
                         ['Sr%d' % j, 'c3'], ['Sr%d' % j, 'pb0'])
            for h in range(4):
                self.cp('act', ost[b][:, h * 64:(h + 1) * 64], self.pb[3 + h][:, 0:64], [], [bk('rost'), 'pb%d' % (3 + h)])
            self.dma('sp', self.o_d[row0:row0 + 128, 512:768], ost[b], [bk('rost')], ['o_d'])
    self.phase_end()


KB.m3 = _m3


def _m4(self, l):
    T = self.T
    SEG = min(512, T)
    nseg = T // SEG
    tps = SEG // 128
    A = self.alloc
    C = 'c4'
    m_strict = A("m_strict", 128); m_inclT = A("m_inclT", 128); m_same = A("m_same", 128); ones_bd = A("ones_bd", 128)
    rmask = A("rmask4", SEG); colA = A("colA4", SEG); colB = A("colB4", SEG)
    for ap, nm in [(m_strict, 'm_strict'), (m_inclT, 'm_incl_T'), (m_same, 'm_same'), (ones_bd, 'ones_bd')]:
        self.dma('sp', ap, self.cd[nm], [], [C])
    self.dma('act', rmask, self.cd['resetmask'][:, 0:SEG], [], [C])
    self.dma('act', colA, self.cd['colA'][:, 0:SEG], [], [C])
    self.dma('act', colB, self.cd['colB'][:, 0:SEG], [], [C])
    cwf = A("cw", 24)
    for jj in range(4):
        self.dma_nc('sp', cwf[:, jj * 6:(jj + 1) * 6], self.conv_w[l][jj].rearrange("(t p) -> p t", p=128), [], [C])
    cwv = lambda ct, jj: cwf[:, jj * 6 + ct:jj * 6 + ct + 1]
    dtb4 = A("dtb4", 1); negA4 = A("negA4", 1); dtbB = A("dtbB", 4); negAB = A("negAB", 4)
    self.dma_nc('sp', dtb4[0:4, :], self.dt_bias[l].rearrange("(h o) -> h o", o=1), [], [C])
    self.dma_nc('sp', negA4[0:4, :], self.A_log[l].rearrange("(h o) -> h o", o=1), [], ['negA4'])
    self.act(negA4[0:4, :], negA4[0:4, :], AF.Exp, ['negA4'], ['negA4'])
    self.ts('dve', negA4[0:4, :], negA4[0:4, :], -1.0, None, ALU.mult, None, ['negA4'], ['negA4'])
    self.dma('act', dtbB, self.dt_bias[l].partition_broadcast(128), [], [C])
    self.dma('act', negAB, self.A_log[l].partition_broadcast(128), [], ['negAB'])
    self.act(negAB, negAB, AF.Exp, ['negAB'], ['negAB'])
    self.ts('dve', negAB, negAB, -1.0, None, ALU.mult, None, ['negAB'], ['negAB'])
    S2 = A("S2g", 128).rearrange("p (j e) -> p j e", e=64)
    self.memset('pool', S2, 0.0, ['Sg0', 'Sg1'])
    qnT = A("qnT", 2 * SEG).rearrange("p (j t) -> p j t", t=SEG)
    knT = A("knT", 2 * SEG).rearrange("p (j t) -> p j t", t=SEG)
    vT = A("vT", 2 * SEG).rearrange("p (j t) -> p j t", t=SEG)
    Gbc = A("Gbc", 4 * SEG).rearrange("p (h t) -> p h t", t=SEG)
    EGs = A("EGs", 2 * SEG).rearrange("p (j t) -> p j t", t=SEG)
    qd = A("qd", 2 * SEG).rearrange("p (j t) -> p j t", t=SEG)
    qdA = A("qdA", 2 * SEG).rearrange("p (j t) -> p j t", t=SEG)
    qdB = A("qdB", 2 * SEG).rearrange("p (j t) -> p j t", t=SEG)
    u = A("u", SEG + 3); acc = A("acc", SEG); xs = A("xs", SEG); sq = A("sq", SEG); rn = A("rn", SEG)
    arow = A("arow", SEG); grow = A("grow", SEG)
    ab = A("ab", tps * 8).rearrange("p (t n) -> p t n", n=8)
    gtm = A("gtm", tps * 4).rearrange("p (t n) -> p t n", n=4)
    gcT = A("gcT", tps * 4).rearrange("p (t n) -> p t n", n=4)
    glT = A("glT", tps * 4).rearrange("p (t n) -> p t n", n=4)
    beta = A("beta", tps * 4).rearrange("p (t n) -> p t n", n=4)
    nbeta = A("nbeta", tps * 4).rearrange("p (t n) -> p t n", n=4)
    bke = A("bke", tps * 4).rearrange("p (t n) -> p t n", n=4)
    kds = A("kds", tps * 4).rearrange("p (t n) -> p t n", n=4)
    ktok = A("ktok", 256); vtok = A("vtok", 256)
    Y = [A("Y%d" % i, 512).rearrange("p (h c) -> p h c", c=128) for i in range(2)]
    Pm = [A("Pm%d" % i, 512).rearrange("p (h c) -> p h c", c=128) for i in range(2)]
    PT = [A("PT%d" % i, 512).rearrange("p (h c) -> p h c", c=128) for i in range(2)]
    kdpad = A("kdpad", 512)
    self.memset('pool', kdpad, 0.0, ['kdpad'])
    A1 = A("A1", 512).rearrange("p (h c) -> p h c", c=128)
    A2 = A("A2", 512).rearrange("p (h c) -> p h c", c=128)
    tmp = A("tmp4", 512).rearrange("p (h c) -> p h c", c=128)
    tmp2 = A("tmp42", 512).rearrange("p (h c) -> p h c", c=128)
    qkTm = A("qkTm", 512).rearrange("p (h c) -> p h c", c=128)
    kcc = A("kcc", 256)
    kcTA = A("kcTA", 256).rearrange("p (j t) -> p j t", t=128)
    kcTB = A("kcTB", 256).rearrange("p (j t) -> p j t", t=128)
    self.memset('pool', kcTA, 0.0, ['kcTA'])
    self.memset('pool', kcTB, 0.0, ['kcTB'])
    vnew = A("vnew", 256).rearrange("p (h e) -> p h e", e=64)
    ost = A("gost", 256)
    par = lambda x3, i: x3.rearrange("p (j i) c -> p j i c", i=2)[:, :, i, :]
    for sg in range(nseg):
        c0 = sg * SEG
        for ct in range(6):
            r0 = 1024 + ct * 128
            if sg == 0:
                self.memset('pool', u[:, 0:3], 0.0, ['u'])
            else:
                self.dma_nc('act', u[:, 0:3], self.zT_d[r0:r0 + 128, c0 - 3:c0], [], ['u'])
            self.dma('sp', u[:, 3:SEG + 3], self.zT_d[r0:r0 + 128, c0:c0 + SEG], [], ['u'])
            self.ts('dve', acc, u[:, 3:SEG + 3], cwv(ct, 3), None, ALU.mult, None, ['u', C], ['acc'])
            for jj in (2, 1, 0):
                self.stt(acc, u[:, jj:jj + SEG], cwv(ct, jj), acc, ALU.mult, ALU.add, ['u', C, 'acc'], ['acc'])
            if ct >= 4:
                self.act(vT[:, ct - 4, :], acc, AF.Silu, ['acc'], ['vT'])
                continue
            self.act(xs, acc, AF.Silu, ['acc'], ['xs'])
            self.tt('pool', sq, xs, xs, ALU.mult, ['xs'], ['sq'])
            bank = ct % 2
            self.mm(self.pb[bank][:, 0:SEG], ones_bd, sq, True, True, [C, 'sq'], ['pb%d' % bank])
            self.act(rn, self.pb[bank][:, 0:SEG], AF.Sqrt, [], ['rn', 'pb%d' % bank], bias=1e-6)
            self.P.op('dve', lambda e: e.reciprocal(out=rn, in_=rn), reads=['rn'], writes=['rn'])
            dst = qnT[:, ct, :] if ct < 2 else knT[:, ct - 2, :]
            self.stt(dst, xs, 0.125 if ct < 2 else 1.0, rn, ALU.mult, ALU.mult, ['xs', 'rn'], ['qnT' if ct < 2 else 'knT'])
        self.dma('sp', arow[0:4, :], self.zT_d[1792:1796, c0:c0 + SEG], [], ['arow'])
        self.act(arow[0:4, :], arow[0:4, :], AF.Exp, ['arow', C], ['arow'], bias=dtb4[0:4, :])
        self.act(arow[0:4, :], arow[0:4, :], AF.Ln, ['arow'], ['arow'], bias=1.0)
        self.ts('dve', arow[0:4, :], arow[0:4, :], negA4[0:4, :], None, ALU.mult, None, ['arow', 'negA4'], ['arow'])
        self.P.op('dve', lambda e: e.tensor_tensor_scan(out=grow[0:4, :], data0=rmask[0:4, :], data1=arow[0:4, :], initial=0.0,
                                                       op0=ALU.mult, op1=ALU.add), reads=['arow', C], writes=['grow'])
        self.dma('sp', self.gcrow_d[0:4, c0:c0 + SEG], grow[0:4, :], ['grow'], ['gcrow_d'])
        for h in range(4):
            self.dma('sp' if h % 2 == 0 else 'act', Gbc[:, h, :], self.gcrow_d[h, c0:c0 + SEG].partition_broadcast(128), ['gcrow_d'], ['Gbc'])
        for j in range(2):
            for i in range(2):
                self.act(EGs[i * 64:(i + 1) * 64, j, :], Gbc[i * 64:(i + 1) * 64, 2 * j + i, :], AF.Exp, ['Gbc'], ['EGs'])
        self.tt('dve', qd, qnT, EGs, ALU.mult, ['qnT', 'EGs'], ['qd'])
        self.tt('pool', qdA, qd, colA.unsqueeze(1).to_broadcast([128, 2, SEG]), ALU.mult, ['qd', C], ['qdA'])
        self.tt('pool', qdB, qd, colB.unsqueeze(1).to_broadcast([128, 2, SEG]), ALU.mult, ['qd', C], ['qdB'])
        self.dma_nc('sp', ab, self.zK_d[c0:c0 + SEG, 2304:2312].rearrange("(t p) n -> p t n", p=128), [], ['ab'])
        self.tt('dve', gtm, ab[:, :, 0:4], dtbB.unsqueeze(1).to_broadcast([128, tps, 4]), ALU.add, ['ab', C], ['gtm'])
        self.act(gtm, gtm, AF.Exp, ['gtm'], ['gtm'])
        self.act(gtm, gtm, AF.Ln, ['gtm'], ['gtm'], bias=1.0)
        self.tt('dve', gtm, gtm, negAB.unsqueeze(1).to_broadcast([128, tps, 4]), ALU.mult, ['gtm', 'negAB'], ['gtm'])
        g2 = gtm.rearrange("p t n -> p (t n)")
        self.mm(self.pb[2][:, 0:tps * 4], m_inclT, g2, True, True, [C, 'gtm'], ['pb2'])
        self.mm(self.pb[3][:, 0:tps * 4], m_same, g2, True, True, [C, 'gtm'], ['pb3'])
        self.cp('dve', gcT.rearrange("p t n -> p (t n)"), self.pb[2][:, 0:tps * 4], [], ['gcT', 'pb2'])
        self.cp('dve', glT.rearrange("p t n -> p (t n)"), self.pb[3][:, 0:tps * 4], [], ['glT', 'pb3'])
        self.act(beta, ab[:, :, 4:8], AF.Sigmoid, ['ab'], ['beta'])
        self.ts('dve', nbeta, beta, -1.0, None, ALU.mult, None, ['beta'], ['nbeta'])
        self.act(bke, gcT, AF.Exp, ['gcT'], ['bke'])
        self.tt('dve', bke, bke, beta, ALU.mult, ['bke', 'beta'], ['bke'])
        self.tt('dve', kds, glT, gcT, ALU.subtract, ['glT', 'gcT'], ['kds'])
        self.act(kds, kds, AF.Exp, ['kds'], ['kds'])
        for ti in range(tps):
            tk = slice(ti * 128, (ti + 1) * 128)
            row0 = c0 + ti * 128
            p0 = self.pb[0].rearrange("p (r t) -> p r t", t=128)
            for j in range(2):
                self.tr(p0[:, j, :], knT[:, j, tk], ['knT'], ['pb0'])
                self.tr(p0[:, 2 + j, :], vT[:, j, tk], ['vT'], ['pb0'])
            self.cp('act', ktok, self.pb[0][:, 0:256], [], ['ktok', 'pb0'])
            self.cp('act', vtok, self.pb[0][:, 256:512], [], ['vtok', 'pb0'])
            Y0 = Y[0]
            self.tt('dve', Y0[:, :, 0:64], vtok.rearrange("p (h d) -> p h d", d=64), beta[:, ti, :].unsqueeze(2).to_broadcast([128, 4, 64]),
                    ALU.mult, ['vtok', 'beta'], ['Y0'])
            self.tt('dve', Y0[:, :, 64:128], ktok.rearrange("p (h d) -> p h d", d=64), bke[:, ti, :].unsqueeze(2).to_broadcast([128, 4, 64]),
                    ALU.mult, ['ktok', 'bke'], ['Y0'])
            k4 = ktok.rearrange("p (j i d) -> p j i d", j=2, i=2)
            Kd4 = kdpad.rearrange("p (j i c) -> p j i c", j=2, i=2)
            kds3 = kds[:, ti, :].rearrange("p (j i) -> p j i", i=2)
            for i in range(2):
                self.tt('pool', Kd4[:, :, i, i * 64:(i + 1) * 64], k4[:, :, i, :], kds3[:, :, i:i + 1].to_broadcast([128, 2, 64]), ALU.mult,
                        ['ktok', 'kds'], ['kdpad'])
            for h in range(4):
                j, i = h // 2, h % 2
                hp = i * 64
                self.mm(self.pb[1 + i][:, j * 128:(j + 1) * 128], knT[hp:hp + 64, j, tk], knT[hp:hp + 64, j, tk], True, True, ['knT'], ['pb%d' % (1 + i)])
                self.mm(self.pb[3 + i][:, j * 128:(j + 1) * 128], knT[hp:hp + 64, j, tk], qnT[hp:hp + 64, j, tk], True, True, ['knT', 'qnT'], ['pb%d' % (3 + i)])
            for h in range(4):
                self.ts('dve', A1[:, h, :], Gbc[:, h, tk], gcT[:, ti, h:h + 1], 0.0, ALU.subtract, ALU.max, ['Gbc', 'gcT'], ['A1'])
                self.ts('dve', A2[:, h, :], Gbc[:, h, tk], gcT[:, ti, h:h + 1], 0.0, ALU.subtract, ALU.min, ['Gbc', 'gcT'], ['A2'])
            self.act(A1, A1, AF.Exp, ['A1'], ['A1'], scale=-1.0)
            self.act(A2, A2, AF.Exp, ['A2'], ['A2'])
            for i in range(2):
                g3 = self.pb[1 + i][:, 0:256].rearrange("p (j c) -> p j c", c=128)
                self.tt('dve', par(tmp, i), g3, par(A1, i), ALU.mult, ['A1'], ['tmp4', 'pb%d' % (1 + i)])
                q3 = self.pb[3 + i][:, 0:256].rearrange("p (j c) -> p j c", c=128)
                self.tt('dve', par(tmp2, i), q3, par(A2, i), ALU.mult, ['A2'], ['tmp42', 'pb%d' % (3 + i)])
            P0 = Pm[0]
            for h in range(4):
                self.stt(P0[:, h, :], tmp[:, h, :], nbeta[:, ti, h:h + 1], m_strict, ALU.mult, ALU.mult, ['tmp4', 'nbeta', C], ['Pm0'])
            self.tt('pool', qkTm, tmp2, m_inclT.unsqueeze(1).to_broadcast([128, 4, 128]), ALU.mult, ['tmp42', C], ['qkTm'])
            p5 = self.pb[5].rearrange("p (r t) -> p r t", t=128)
            for h in range(4):
                self.tr(p5[:, h, :], P0[:, h, :], ['Pm0'], ['pb5'])
            self.cp('act', PT[0], p5, [], ['PT0', 'pb5'])
            cur = 0
            for lev in range(6):
                nxt = 1 - cur
                p6 = self.pb[6].rearrange("p (r t) -> p r t", t=128)
                for h in range(4):
                    self.mm(p6[:, h, :], PT[cur][:, h, :], Y[cur][:, h, :], True, True, ['PT%d' % cur, 'Y%d' % cur], ['pb6'])
                self.tt('dve', Y[nxt], Y[cur], p6, ALU.add, ['Y%d' % cur], ['Y%d' % nxt, 'pb6'])
                if lev < 5:
                    p7 = self.pb[7].rearrange("p (r t) -> p r t", t=128)
                    for h in range(4):
                        self.mm(p7[:, h, :], PT[cur][:, h, :], Pm[cur][:, h, :], True, True, ['PT%d' % cur, 'Pm%d' % cur], ['pb7'])
                    for h in range(4):
                        self.mm(p5[:, h, :], Pm[cur][:, h, :], PT[cur][:, h, :], True, True, ['PT%d' % cur, 'Pm%d' % cur], ['pb5'])
                    self.cp('act', Pm[nxt], p7, [], ['Pm%d' % nxt, 'pb7'])
                    self.cp('act', PT[nxt], p5, [], ['PT%d' % nxt, 'pb5'])
                cur = nxt
            Yf = Y[cur]
            YK = 'Y%d' % cur
            self.cp('pool', kcc.rearrange("p (h d) -> p h d", d=64), Yf[:, :, 64:128], [YK], ['kcc'])
            for j in range(2):
                self.tr(p0[:, j, :], kcc[:, j * 128:(j + 1) * 128], ['kcc'], ['pb0'])
            self.cp('act', kcTA[:, :, 0:64], p0[:, 0:2, 0:64], [], ['kcTA', 'pb0'])
            self.cp('act', kcTB[:, :, 64:128], p0[:, 0:2, 64:128], [], ['kcTB', 'pb0'])
            for h in range(4):
                j, i = h // 2, h % 2
                hp = i * 64
                self.mm(self.pb[4 + h][:, 0:64], qdA[hp:hp + 64, j, tk], S2[hp:hp + 64, j, :], True, False, ['qdA', 'Sg%d' % j], ['pb%d' % (4 + h)])
                self.mm(self.pb[1 + i][:, j * 64:(j + 1) * 64], kcTA[hp:hp + 64, j, :], S2[hp:hp + 64, j, :], True, True, ['kcTA', 'Sg%d' % j], ['pb%d' % (1 + i)])
            for i in range(2):
                v3 = self.pb[1 + i][0:64, 0:128].rearrange("p (j e) -> p j e", e=64)
                self.tt('dve', par(vnew, i)[0:64], par(Yf, i)[0:64, :, 0:64], v3, ALU.subtract, [YK], ['vnew', 'pb%d' % (1 + i)])
            for j in range(2):
                for i in range(2):
                    h = 2 * j + i
                    self.mm(self.pb[3][:, j * 64:(j + 1) * 64], kdpad[0:64, h * 128:(h + 1) * 128], vnew[0:64, h, :], i == 0, i == 1,
                            ['kdpad', 'vnew'], ['pb3'])
            for j in range(2):
                cA = (ti * 2) * 64 + 63
                self.stt(S2[:, j, :], S2[:, j, :], EGs[:, j, cA:cA + 1], self.pb[3][:, j * 64:(j + 1) * 64], ALU.mult, ALU.add,
                         ['Sg%d' % j, 'EGs'], ['Sg%d' % j, 'pb3'])
            for h in range(4):
                j, i = h // 2, h % 2
                hp = i * 64
                self.mm(self.pb[4 + h][:, 0:64], qdB[hp:hp + 64, j, tk], S2[hp:hp + 64, j, :], False, False, ['qdB', 'Sg%d' % j], ['pb%d' % (4 + h)])
                self.mm(self.pb[1 + i][:, j * 64:(j + 1) * 64], kcTB[hp:hp + 64, j, :], S2[hp:hp + 64, j, :], True, True, ['kcTB', 'Sg%d' % j], ['pb%d' % (1 + i)])
            for i in range(2):
                v3 = self.pb[1 + i][64:128, 0:128].rearrange("p (j e) -> p j e", e=64)
                self.tt('dve', par(vnew, i)[64:128], par(Yf, i)[64:128, :, 0:64], v3, ALU.subtract, [YK], ['vnew', 'pb%d' % (1 + i)])
            for h in range(4):
                self.mm(self.pb[4 + h][:, 0:64], qkTm[:, h, :], vnew[:, h, :], False, True, ['qkTm', 'vnew'], ['pb%d' % (4 + h)])
            for j in range(2):
                for i in range(2):
                    h = 2 * j + i
                    self.mm(self.pb[3][:, 128 + j * 64:128 + (j + 1) * 64], kdpad[64:128, h * 128:(h + 1) * 128], vnew[64:128, h, :], i == 0, i == 1,
                            ['kdpad', 'vnew'], ['pb3'], fence=(i == 0 and j == 0))
            for j in range(2):
                cB = (ti * 2 + 1) * 64 + 63
                self.stt(S2[:, j, :], S2[:, j, :], EGs[:, j, cB:cB + 1], self.pb[3][:, 128 + j * 64:128 + (j + 1) * 64], ALU.mult, ALU.add,
                         ['Sg%d' % j, 'EGs'], ['Sg%d' % j, 'pb3'])
            for h in range(4):
                self.cp('act', ost[:, h * 64:(h + 1) * 64], self.pb[4 + h][:, 0:64], [], ['gost', 'pb%d' % (4 + h)])
            self.dma('sp', self.o_d[row0:row0 + 128, 768:1024], ost, ['gost'], ['o_d'])
    self.phase_end()


KB.m4 = _m4


def _m5(self, l, xin, xout):
    T, nT = self.T, self.nT
    A = self.alloc
    Wo = A("Wo", 8 * D).rearrange("p (k n) -> p k n", n=D)
    self.dma('sp', Wo, self.w_out[l].rearrange("(k p) n -> p k n", p=128), [], ['Wo'])
    gbc = A("gbc", D); gt1 = A("gt1", D)
    self.dma('act', gbc, self.mixg[l].partition_broadcast(128), [], ['c5'])
    self.dma('act', gt1, self.mod_d[l, 2 * D:3 * D].partition_broadcast(128), [], ['c5'])
    NB = 2
    ot = [A("ot%d" % i, D) for i in range(NB)]
    gtile = [A("gtile%d" % i, D) for i in range(NB)]
    xt = [A("x5%d" % i, D) for i in range(NB)]
    sq = [A("sq5%d" % i, D) for i in range(NB)]
    ss = [A("ss5%d" % i, 12) for i in range(NB)]
    on = [A("on%d" % i, D) for i in range(NB)]
    onT = [A("onT%d" % i, 8 * 128).rearrange("p (k n) -> p k n", n=128) for i in range(NB)]
    yst = [A("y5%d" % i, D) for i in range(NB)]
    def prep5(i):
        b = i % NB
        bk = lambda n: "%s%d" % (n, b)
        r = slice(i * 128, (i + 1) * 128)
        self.dma('sp', ot[b], self.o_d[r, :], [], [bk('ot')])
        self.dma('act', gtile[b][:, 0:512], self.zK_d[r, 512:1024], [], [bk('gtile')])
        self.dma('act', gtile[b][:, 512:1024], self.zK_d[r, 1792:2304], [], [bk('gtile')])
        self.dma('sp', xt[b], xin[r, :], [], [bk('x5')])
        self.tt('pool', sq[b], ot[b], ot[b], ALU.mult, [bk('ot')], [bk('sq5')])
        self.P.op('dve', lambda e, b=b: e.tensor_reduce(out=ss[b][:, 0:4], in_=sq[b][:, 0:512].rearrange("p (h d) -> p h d", d=128), axis=AX.X, op=ALU.add),
                  reads=[bk('sq5')], writes=[bk('ss5')])
        self.P.op('dve', lambda e, b=b: e.tensor_reduce(out=ss[b][:, 4:12], in_=sq[b][:, 512:1024].rearrange("p (h d) -> p h d", d=64), axis=AX.X, op=ALU.add),
                  reads=[bk('sq5')], writes=[bk('ss5')])
        self.ts('dve', ss[b][:, 0:4], ss[b][:, 0:4], 1.0 / 128, 1e-6, ALU.mult, ALU.add, [bk('ss5')], [bk('ss5')])
        self.ts('dve', ss[b][:, 4:12], ss[b][:, 4:12], 1.0 / 64, 1e-6, ALU.mult, ALU.add, [bk('ss5')], [bk('ss5')])
        self.act(ss[b], ss[b], AF.Sqrt, [bk('ss5')], [bk('ss5')])
        self.P.op('dve', lambda e, b=b: e.reciprocal(out=ss[b], in_=ss[b]), reads=[bk('ss5')], writes=[bk('ss5')])
        self.tt('dve', on[b][:, 0:512].rearrange("p (h d) -> p h d", d=128), ot[b][:, 0:512].rearrange("p (h d) -> p h d", d=128),
                ss[b][:, 0:4].unsqueeze(2).to_broadcast([128, 4, 128]), ALU.mult, [bk('ot'), bk('ss5')], [bk('on') + 'hn'])
        self.tt('dve', on[b][:, 512:1024].rearrange("p (h d) -> p h d", d=64), ot[b][:, 512:1024].rearrange("p (h d) -> p h d", d=64),
                ss[b][:, 4:12].unsqueeze(2).to_broadcast([128, 8, 64]), ALU.mult, [bk('ot'), bk('ss5')], [bk('on') + 'hn'])
        self.act(gtile[b], gtile[b], AF.Silu, [bk('gtile')], [bk('gtile')])
        self.tt('pool', on[b], on[b], gbc, ALU.mult, [bk('on') + 'hn', 'c5'], [bk('on') + 'hn'])
        self.tt('dve', on[b], on[b], gtile[b], ALU.mult, [bk('on') + 'hn', bk('gtile')], [bk('on') + 'hn'])


    def tr5(i):
        b = i % NB
        bk = lambda n: "%s%d" % (n, b)
        self.transpose_tile(on[b], onT[b], 0, bk('on'), bk('onT'))

    prep5(0)
    tr5(0)
    for i in range(nT):
        b = i % NB
        bk = lambda n: "%s%d" % (n, b)
        r = slice(i * 128, (i + 1) * 128)
        for half in range(2):
            ps = self.pb[half][:, :]
            for k in range(8):
                self.mm(ps, onT[b][:, k, :], Wo[:, k, half * 512:(half + 1) * 512], k == 0, k == 7, [bk('onT'), 'Wo'], ['pb%d' % half])
            self.tt('dve', yst[b][:, half * 512:(half + 1) * 512], ps, gt1[:, half * 512:(half + 1) * 512], ALU.mult, ['c5'], [bk('y5'), 'pb%d' % half])
            if half == 0 and i + 1 < nT:
                prep5(i + 1)
            if half == 1 and i + 1 < nT:
                tr5(i + 1)
        self.tt('pool', yst[b], yst[b], xt[b], ALU.add, [bk('y5'), bk('x5')], [bk('y5')])
        self.dma('sp', xout[r, :], yst[b], [bk('y5')], ['xout'])
    self.phase_end()


KB.m5 = _m5
KB.moe = lambda self, l, xin, xout, last: None


def _moe(self, l, xin, xout, last):
    T, nT, n_blk = self.T, self.nT, self.n_blk
    A = self.alloc
    gsel = A("gsel", nT * 4).rearrange("p (i k) -> p i k", k=4)
    sloti = A("sloti", nT * 4, I32).rearrange("p (i k) -> p i k", k=4)
    idxW = A("idxW", n_blk * 8, I32).rearrange("p (j k) -> p j k", k=8)
    bidx = A("bidx", n_blk, I32)
    idxH = A("idxH", n_blk * 16, I32).rearrange("p (j k q) -> p j k q", k=8, q=2)
    mark2 = self.aoff
    zt = A("zt", 8192)
    self.memset('pool', zt, 0.0, ['zt'])
    zrows = 128 * 8
    for zi in range(self.PS // zrows):
        self.dma('sp' if zi % 2 == 0 else 'act', self.xg_d[zi * zrows:(zi + 1) * zrows, :].rearrange("(p a) n -> p (a n)", a=8), zt, ['zt'], ['xg_d'])
    G, S = self.load_mod(l, 2, self.norm2_g[l])
    Wr = A("Wr", 8 * NE).rearrange("p (k n) -> p k n", n=NE)
    self.dma('sp', Wr, self.router_w[l].rearrange("(k p) n -> p k n", p=128), [], ['c6'])
    rb = A("rb", NE)
    self.dma('act', rb, self.router_b[l].partition_broadcast(128), [], ['c6'])
    ones = A("ones6", 128); tri = A("tri6", 128)
    self.dma('sp', ones, self.cd['ones'], [], ['c6'])
    self.dma('act', tri, self.cd['tri_strict'], [], ['c6'])
    maskAll = A("maskAll", nT * NE).rearrange("p (i e) -> p i e", e=NE)
    gateAll = A("gateAll", nT * NE).rearrange("p (i e) -> p i e", e=NE)
    NB = 2
    xt = [A("x6%d" % i, D) for i in range(NB)]
    hn = [A("h6%d" % i, D) for i in range(NB)]
    ss = [A("s6%d" % i, 1) for i in range(NB)]
    hnT = [A("hT6%d" % i, 8 * 128).rearrange("p (k n) -> p k n", n=128) for i in range(NB)]
    lg = [A("lg%d" % i, NE) for i in range(NB)]
    ex = [A("ex%d" % i, NE) for i in range(NB)]
    m8 = [A("m8%d" % i, 8) for i in range(NB)]
    sm = [A("sm%d" % i, 2) for i in range(NB)]
    def normE(i):
        b = i % NB
        tg = 'u%d' % b
        r = slice(i * 128, (i + 1) * 128)
        self.norm_tile(xin[r, :], G, S, xt[b], hn[b], ss[b], tg)
        self.dma('act', self.hn_d[r, :], hn[b], [tg + 'hn'], ['hn_d'])

    normE(0)
    for i in range(nT):
        b = i % NB
        bk = lambda n: "%s%d" % (n, b)
        tg = 'u%d' % b
        r = slice(i * 128, (i + 1) * 128)
        self.transpose_tile(hn[b], hnT[b], 0, tg, bk('hT6'))
        ps = self.pb[b][:, 0:NE]
        for k in range(8):
            self.mm(ps, hnT[b][:, k, :], Wr[:, k, :], k == 0, k == 7, [bk('hT6'), 'c6'], ['pb%d' % b])
        if i + 1 < nT:
            normE(i + 1)
        self.tt('dve', lg[b], ps, rb, ALU.add, ['c6'], [bk('lg'), 'pb%d' % b])
        self.P.op('dve', lambda e, b=b: e.max(out=m8[b], in_=lg[b]), reads=[bk('lg')], writes=[bk('m8')])
        self.ts('dve', maskAll[:, i, :], lg[b], m8[b][:, 3:4], None, ALU.is_ge, None, [bk('lg'), bk('m8')], ['maskAll'])
        self.ts('dve', sm[b][:, 0:1], m8[b][:, 0:1], -1.0, None, ALU.mult, None, [bk('m8')], [bk('sm')])
        self.act(ex[b], lg[b], AF.Exp, [bk('lg'), bk('sm')], [bk('ex')], bias=sm[b][:, 0:1])
        self.tt('dve', ex[b], ex[b], maskAll[:, i, :], ALU.mult, [bk('ex'), 'maskAll'], [bk('ex')])
        self.P.op('dve', lambda e, b=b: e.tensor_reduce(out=sm[b][:, 1:2], in_=ex[b], axis=AX.X, op=ALU.add), reads=[bk('ex')], writes=[bk('sm')])
        self.P.op('dve', lambda e, b=b: e.reciprocal(out=sm[b][:, 1:2], in_=sm[b][:, 1:2]), reads=[bk('sm')], writes=[bk('sm')])
        self.ts('dve', gateAll[:, i, :], ex[b], sm[b][:, 1:2], None, ALU.mult, None, [bk('ex'), bk('sm')], ['gateAll'])
    tcnt = A("tcnt", nT * NE).rearrange("p (i e) -> p i e", e=NE)
    rank = A("rank", nT * NE).rearrange("p (i e) -> p i e", e=NE)
    mflat = maskAll.rearrange("p i e -> p (i e)")
    CH = 512
    ncol = nT * NE
    for c in range((ncol + CH - 1) // CH):
        a0, a1 = c * CH, min(ncol, (c + 1) * CH)
        self.mm(self.pb[2][:, 0:a1 - a0], ones, mflat[:, a0:a1], True, True, ['c6', 'maskAll'], ['pb2'])
        self.cp('act', tcnt.rearrange("p i e -> p (i e)")[:, a0:a1], self.pb[2][:, 0:a1 - a0], [], ['tcnt', 'pb2'])
        self.mm(self.pb[3][:, 0:a1 - a0], tri, mflat[:, a0:a1], True, True, ['c6', 'maskAll'], ['pb3'])
        self.cp('dve', rank.rearrange("p i e -> p (i e)")[:, a0:a1], self.pb[3][:, 0:a1 - a0], [], ['rank', 'pb3'])
    tcT = A("tcT", NE * nT).rearrange("p (e i) -> p e i", i=nT)
    incl = A("incl", NE * nT).rearrange("p (e i) -> p e i", i=nT)
    rm2 = A("rm2", NE * nT).rearrange("p (e i) -> p e i", i=nT)
    self.cp('dve', tcT, tcnt.rearrange("p i e -> p e i"), ['tcnt'], ['tcT'])
    self.memset('pool', rm2, 1.0, ['rm2'])
    self.memset('pool', rm2[:, :, 0:1], 0.0, ['rm2'])
    self.P.op('dve', lambda e: e.tensor_tensor_scan(out=incl.rearrange("p e i -> p (e i)"), data0=rm2.rearrange("p e i -> p (e i)"),
                                                   data1=tcT.rearrange("p e i -> p (e i)"), initial=0.0, op0=ALU.mult, op1=ALU.add),
              reads=['tcT', 'rm2'], writes=['incl'])
    tot = A("tot", NE); pad = A("pad", NE); padi = A("padi", NE, I32); pend = A("pend", NE); pst = A("pst", NE); one32 = A("one32", NE)
    self.cp('dve', tot, incl[:, :, nT - 1], ['incl'], ['tot'])
    self.ts('dve', pad, tot, float(EB - 1), None, ALU.add, None, ['tot'], ['pad'])
    self.cp('dve', padi, pad, ['pad'], ['padi'])
    self.ts('dve', padi, padi, 9, 9, ALU.arith_shift_right, ALU.logical_shift_left, ['padi'], ['padi'])
    self.cp('dve', pad, padi, ['padi'], ['pad'])
    self.memset('pool', one32, 1.0, ['one32'])
    self.P.op('dve', lambda e: e.tensor_tensor_scan(out=pend, data0=one32, data1=pad, initial=0.0, op0=ALU.mult, op1=ALU.add),
              reads=['pad', 'one32'], writes=['pend'])
    self.tt('dve', pst, pend, pad, ALU.subtract, ['pend', 'pad'], ['pst'])
    self.tt('dve', incl, incl, tcT, ALU.subtract, ['incl', 'tcT'], ['incl'])
    self.tt('dve', incl, incl, pst.unsqueeze(2).to_broadcast([128, NE, nT]), ALU.add, ['incl', 'pst'], ['incl'])
    self.tt('dve', rank, rank, incl.rearrange("p e i -> p i e"), ALU.add, ['rank', 'incl'], ['rank'])
    self.stt(rank, rank, 1.0, maskAll, ALU.add, ALU.mult, ['rank', 'maskAll'], ['rank'])
    jg = A("jg", n_blk * NE).rearrange("p (j e) -> p j e", e=NE)
    self.dma('sp', jg, self.cd['jgrid'], [], ['jg'])
    bef = A("bef", n_blk)
    self.tt('dve', jg, pend.unsqueeze(1).to_broadcast([128, n_blk, NE]), jg, ALU.is_le, ['pend', 'jg'], ['jg'])
    self.P.op('dve', lambda e: e.tensor_reduce(out=bef, in_=jg, axis=AX.X, op=ALU.add), reads=['jg'], writes=['bef'])
    self.ts('dve', bef, bef, float(NE - 1), float(l * NE), ALU.min, ALU.add, ['bef'], ['bef'])
    self.cp('dve', bidx, bef, ['bef'], ['bidx'])
    ib = A("ib", 8)
    self.dma('act', ib, self.cd['idxbase'], [], ['ib'])
    idf = A("idf", n_blk * 8).rearrange("p (j k) -> p j k", k=8)
    self.ts('dve', bef, bef, float(D), None, ALU.mult, None, ['bef'], ['bef'])
    self.tt('dve', idf, bef.unsqueeze(2).to_broadcast([128, n_blk, 8]), ib.unsqueeze(1).to_broadcast([128, n_blk, 8]), ALU.add,
            ['bef', 'ib'], ['idf'])
    self.cp('dve', idxW, idf, ['idf'], ['idxW'])
    idq = A("idq", n_blk * 16).rearrange("p (j k q) -> p j k q", k=8, q=2)
    for q in range(2):
        self.ts('dve', idq[:, :, :, q], idf, 2.0, float(q), ALU.mult, ALU.add, ['idf'], ['idq'])
    self.cp('dve', idxH.rearrange("p j k q -> p (j k q)"), idq.rearrange("p j k q -> p (j k q)"), ['idq'], ['idxH'])
    slotf = A("slotf", nT * 4).rearrange("p (i k) -> p i k", k=4)
    oh = [A("oh%d" % i, NE) for i in range(NB)]
    for i in range(nT):
        b = i % NB
        bk = lambda n: "%s%d" % (n, b)
        r = slice(i * 128, (i + 1) * 128)
        self.P.op('dve', lambda e, b=b, i=i: e.max(out=m8[b], in_=rank[:, i, :]), reads=['rank'], writes=[bk('m8')])
        for k in range(4):
            self.ts('dve', oh[b], rank[:, i, :], m8[b][:, k:k + 1], None, ALU.is_equal, None, ['rank', bk('m8')], [bk('oh')])
            self.tt('dve', oh[b], oh[b], gateAll[:, i, :], ALU.mult, [bk('oh'), 'gateAll'], [bk('oh')])
            self.P.op('dve', lambda e, b=b, i=i, k=k: e.tensor_reduce(out=gsel[:, i, k:k + 1], in_=oh[b], axis=AX.X, op=ALU.add),
                      reads=[bk('oh')], writes=['gsel'])
        self.ts('dve', slotf[:, i, :], m8[b][:, 0:4], -1.0, None, ALU.add, None, [bk('m8')], ['slotf'])
        self.cp('dve', sloti[:, i, :], slotf[:, i, :], ['slotf'], ['sloti%d' % i])
        self.dma('sp', hn[b], self.hn_d[r, :], ['hn_d'], [bk('h6')])
        for k in range(4):
            self.scatter(self.xg_d, hn[b], sloti[:, i, k:k + 1], [bk('h6'), 'sloti%d' % i], ['xg_d'])
    self.P.barrier()
    self.aoff = mark2
    w1q = self.w1.rearrange("r (q c) -> (r q) c", q=2)
    W1h = [A("W1A", 8 * 1024).rearrange("p (k n) -> p k n", n=1024), A("W1B", 8 * 1024).rearrange("p (k n) -> p k n", n=1024)]
    W2 = A("W2", 8 * D).rearrange("p (k n) -> p k n", n=D)
    b1bc = A("b1bc", 2 * D); b2bc = A("b2bc", D)
    onesr = A("onesr", 8)
    self.memset('pool', onesr, 1.0, ['onesr'])
    b1col = A("b1col", 16); b1p1 = A("b1p1", 8)
    xg = [A("xg%d" % i, D) for i in range(2)]
    xgT = A("xgT", 8 * EB).rearrange("p (k n) -> p k n", n=EB)
    actT = A("actT", 8 * EB).rearrange("p (k n) -> p k n", n=EB)
    gtl = [A("gtl%d" % i, EB) for i in range(2)]
    sgl = [A("sgl%d" % i, EB) for i in range(2)]
    ltl = [A("ltl%d" % i, EB) for i in range(2)]
    yst = [A("yst%d" % i, 512) for i in range(2)]
    nrt = EB // 128
    for j in range(n_blk):
        for hf in range(2):
            for k in range(8):
                self.gather(W1h[hf][:, k, :], w1q, idxH[:, j, k, hf:hf + 1], ['idxH'], ['W1%d' % hf])
        self.gather(b1bc, self.b1, bidx[:, j:j + 1], ['bidx'], ['b1bc'])
        for k in range(8):
            self.gather(W2[:, k, :], self.w2, idxW[:, j, k:k + 1], ['idxW'], ['W2'])
        self.gather(b2bc, self.b2, bidx[:, j:j + 1], ['bidx'], ['b2bc'])
        for m in range(16):
            mq = m % 8
            bc0 = (mq // 4) * 1024 + (m // 8) * 512 + (mq % 4) * 128
            self.mm(self.pb[5][:, m:m + 1], b1bc[0:1, bc0:bc0 + 128], onesr[0:1, 0:1], True, True, ['b1bc', 'onesr'], ['pb5'])
        self.cp('act', b1col, self.pb[5][:, 0:16], [], ['b1col', 'pb5'])
        self.ts('dve', b1p1, b1col[:, 8:16], 1.0, None, ALU.add, None, ['b1col'], ['b1p1'])
        for rr in range(nrt):
            b = rr % 2
            r0 = j * EB + rr * 128
            self.dma('sp' if rr % 2 == 0 else 'act', xg[b], self.xg_d[r0:r0 + 128, :], ['xg_d'], ['xg%dhn' % b])
            self.transpose_tile(xg[b], xgT, rr * 128, 'xg%d' % b, 'xgT')
        for m in range(8):
            hf, mm_ = m // 4, m % 4
            pp = m % 2
            banks = (0, 1) if pp == 0 else (2, 3)
            for part in range(2):
                bank = banks[part]
                ps = self.pb[bank][:, 0:EB]
                c0 = part * 512 + mm_ * 128
                for k in range(8):
                    self.mm(ps, W1h[hf][:, k, c0:c0 + 128], xgT[:, k, :], k == 0, k == 7, ['W1%d' % hf, 'xgT'], ['pb%d' % bank])
            g_, s_, l_ = gtl[pp], sgl[pp], ltl[pp]
            gk, sk, lk = 'gtl%d' % pp, 'sgl%d' % pp, 'ltl%d' % pp
            self.ts('dve', g_, self.pb[banks[0]][:, 0:EB], b1col[:, m:m + 1], 7.0, ALU.add, ALU.min, ['b1col'], [gk, 'pb%d' % banks[0]])
            self.act(s_, g_, AF.Sigmoid, [gk], [sk], scale=1.702)
            self.ts('dve', l_, self.pb[banks[1]][:, 0:EB], b1p1[:, m:m + 1], -6.0, ALU.add, ALU.max, ['b1p1'], [lk, 'pb%d' % banks[1]])
            self.tt('dve', s_, s_, g_, ALU.mult, [sk, gk], [sk])
            self.stt(actT[:, m, :], l_, 8.0, s_, ALU.min, ALU.mult, [lk, sk], ['actT'])
        for rr in range(nrt):
            r0 = j * EB + rr * 128
            for m in range(8):
                for half in range(2):
                    self.mm(self.pb[6 + half][:, :], actT[:, m, rr * 128:(rr + 1) * 128], W2[:, m, half * 512:(half + 1) * 512], m == 0, m == 7,
                            ['actT', 'W2'], ['pb%d' % (6 + half)])
            for half in range(2):
                self.tt('dve', yst[half], self.pb[6 + half][:, :], b2bc[:, half * 512:(half + 1) * 512], ALU.add, ['b2bc'], ['yst%d' % half, 'pb%d' % (6 + half)])
                self.dma('sp' if half == 0 else 'act', self.yg_d[r0:r0 + 128, half * 512:(half + 1) * 512], yst[half], ['yst%d' % half], ['yg_d'])
    self.P.barrier()
    self.aoff = mark2
    gt2 = A("gt2", D)
    self.dma('act', gt2, self.mod_d[l, 5 * D:6 * D].partition_broadcast(128), [], ['c7'])
    fg = A("fg", D)
    if last:
        self.dma('act', fg, self.fin_g.partition_broadcast(128), [], ['c7'])
    yr = [A("yr%d" % i, D) for i in range(4)]
    accb = [A("acc7%d" % i, D) for i in range(2)]
    x1 = [A("x7%d" % i, D) for i in range(2)]
    ob = [A("o7%d" % i, D) for i in range(2)]
    s7 = [A("s7%d" % i, 1) for i in range(2)]
    for i in range(nT):
        b = i % 2
        bk = lambda n: "%s%d" % (n, b)
        r = slice(i * 128, (i + 1) * 128)
        self.dma('sp', x1[b], xin[r, :], [], [bk('x7')])
        for k in range(4):
            self.gather(yr[k], self.yg_d, sloti[:, i, k:k + 1], ['yg_d'], ['yr%d' % k])
        self.ts('dve', accb[b], yr[0], gsel[:, i, 0:1], None, ALU.mult, None, ['yr0'], [bk('acc7')])
        for k in range(1, 4):
            self.stt(accb[b], yr[k], gsel[:, i, k:k + 1], accb[b], ALU.mult, ALU.add, ['yr%d' % k, bk('acc7')], [bk('acc7')])
        self.tt('pool', accb[b], accb[b], gt2, ALU.mult, [bk('acc7'), 'c7'], [bk('acc7')])
        self.tt('pool', accb[b], accb[b], x1[b], ALU.add, [bk('acc7'), bk('x7')], [bk('acc7')])
        if not last:
            self.dma('sp', xout[r, :], accb[b], [bk('acc7')], ['xout'])
        else:
            if self.dbg:
                self.dma('act', xout[r, :], accb[b], [bk('acc7')], ['xout'])
            self.act(ob[b], accb[b], AF.Square, [bk('acc7')], [bk('o7'), bk('s7')], accum=s7[b])
            self.ts('dve', s7[b], s7[b], 1.0 / D, 1e-6, ALU.mult, ALU.add, [bk('s7')], [bk('s7')])
            self.act(s7[b], s7[b], AF.Sqrt, [bk('s7')], [bk('s7')])
            self.P.op('dve', lambda e, b=b: e.reciprocal(out=s7[b], in_=s7[b]), reads=[bk('s7')], writes=[bk('s7')])
            self.stt(ob[b], accb[b], s7[b], fg, ALU.mult, ALU.mult, [bk('acc7'), bk('s7'), 'c7', bk('o7')], [bk('o7')])
            self.dma('sp', self.out_d[r, :], ob[b], [bk('o7')], ['out'])
    self.phase_end()


KB.moe = _moe


_CACHE = {}


def kernel(**inputs):
    B, S, _ = inputs["x"].shape
    T = S
    if T not in _CACHE:
        _CACHE[T] = KB(T)
    kb = _CACHE[T]
    base = prep_inputs(inputs, 0, T, 0, kb.n_blk)
    in_maps = []
    for b in range(B):
        m = dict(base)
        m["x"] = np.ascontiguousarray(inputs["x"][b])
        m["cT"] = np.ascontiguousarray(inputs["c"][b].reshape(8, 128).T)
        in_maps.append(m)
    res = run_bass_kernel_spmd(kb.nc, in_maps, core_ids=list(range(B)))
    out = np.stack([np.asarray(r["out"], dtype=np.float32) for r in res.results], axis=0)
    return out
```

```python
import numpy as np
import os
from contextlib import ExitStack
import concourse.bass as bass
import concourse.mybir as mybir
from concourse.bass_utils import run_bass_kernel_spmd

F32 = mybir.dt.float32
I32 = mybir.dt.int32
U32 = mybir.dt.uint32
AF = mybir.ActivationFunctionType
ALU = mybir.AluOpType
AX = mybir.AxisListType

D = 1024
N_IN = 4104
NE = 32
DEPTH = 2
EB = 512

ENG_NAMES = ['pe', 'act', 'dve', 'pool', 'sp']
SAME_ENGINE_SYNC = {'pe': False, 'act': True, 'dve': True, 'pool': True, 'sp': False}
EPOCH = 20000


class Prog:
    def __init__(self, nc, stack, n_dma_sems=12):
        self.nc = nc
        self.stack = stack
        self.q = {e: [] for e in ENG_NAMES}
        self.semobjs = {}
        self.epoch = {e: 0 for e in ENG_NAMES}
        self.cnt = {e: 0 for e in ENG_NAMES}
        self.cursem = {}
        for e in ENG_NAMES:
            self._new_epoch(e, first=True)
        self.waited = {e: {} for e in ENG_NAMES}
        self.dsems = {}
        for e in ['sp', 'act', 'pool']:
            self.dsems[e] = []
            for i in range(n_dma_sems):
                nm = "d_%s_%d" % (e, i)
                s = stack.enter_context(nc.semaphore(nm))
                self.semobjs[nm] = s
                self.dsems[e].append([nm, 0])
        self.drr = {e: 0 for e in self.dsems}
        self.last_w = {}
        self.readers = {}
        self.n_inst = 0

    def _new_epoch(self, e, first=False):
        if not first:
            self.epoch[e] += 1
        nm = "s_%s_%d" % (e, self.epoch[e])
        self.semobjs[nm] = self.stack.enter_context(self.nc.semaphore(nm))
        self.cursem[e] = nm
        self.cnt[e] = 0

    def _wait(self, eng, tok):
        if tok is None:
            return
        sname, val, teng = tok
        if teng == eng and not SAME_ENGINE_SYNC[eng]:
            return
        if self.waited[eng].get(sname, 0) >= val:
            return
        self.waited[eng][sname] = val
        s = self.semobjs[sname]
        self.q[eng].append(lambda e, s=s, val=val: e.wait_ge(s, val))

    def _deps(self, eng, reads, writes):
        for k in reads:
            self._wait(eng, self.last_w.get(k))
        for k in writes:
            self._wait(eng, self.last_w.get(k))
            for t in self.readers.get(k, ()):
                self._wait(eng, t)

    def _commit(self, tok, reads, writes):
        for k in reads:
            self.readers.setdefault(k, []).append(tok)
        for k in writes:
            self.last_w[k] = tok
            self.readers[k] = []

    def op(self, eng, fn, reads=(), writes=()):
        self._deps(eng, reads, writes)
        if self.cnt[eng] >= EPOCH:
            self._new_epoch(eng)
        self.cnt[eng] += 1
        val = self.cnt[eng]
        nm = self.cursem[eng]
        s = self.semobjs[nm]
        self.q[eng].append(lambda e, fn=fn, s=s: fn(e).then_inc(s, 1))
        self._commit((nm, val, eng), reads, writes)
        self.n_inst += 1

    def dma(self, eng, fn, reads=(), writes=()):
        self._deps(eng, reads, writes)
        i = self.drr[eng]
        self.drr[eng] = (i + 1) % len(self.dsems[eng])
        ent = self.dsems[eng][i]
        if ent[1] > 0:
            self._wait(eng, (ent[0], ent[1], None))
        ent[1] += 16
        s = self.semobjs[ent[0]]
        self.q[eng].append(lambda e, fn=fn, s=s: fn(e).then_inc(s, 16))
        self._commit((ent[0], ent[1], None), reads, writes)
        self.n_inst += 1

    def fence(self, eng):
        if self.cnt[eng] == 0:
            return
        nm = self.cursem[eng]
        val = self.cnt[eng]
        if self.waited[eng].get(nm, 0) >= val:
            return
        self.waited[eng][nm] = val
        s = self.semobjs[nm]
        self.q[eng].append(lambda e, s=s, val=val: e.wait_ge(s, val))

    def barrier(self):
        toks = []
        for e in ENG_NAMES:
            if self.cnt[e] > 0:
                toks.append((self.cursem[e], self.cnt[e], e))
        for e in self.dsems:
            for nm, v in self.dsems[e]:
                if v > 0:
                    toks.append((nm, v, None))
        for e in ENG_NAMES:
            for t in toks:
                if t[2] != e:
                    self._wait(e, t)
        self.last_w = {}
        self.readers = {}

    def emit(self):
        with self.nc.Block() as block:
            @block.sync
            def _(e):
                for f in self.q['sp']:
                    f(e)

            @block.scalar
            def _(e):
                for f in self.q['act']:
                    f(e)

            @block.vector
            def _(e):
                for f in self.q['dve']:
                    f(e)

            @block.gpsimd
            def _(e):
                for f in self.q['pool']:
                    f(e)

            @block.tensor
            def _(e):
                for f in self.q['pe']:
                    f(e)


def make_consts(T, pos0, n_blk):
    c = {}
    c['ident'] = np.eye(128, dtype=np.float32)
    i = np.arange(128)
    same = (i[:, None] // 64) == (i[None, :] // 64)
    le = i[:, None] <= i[None, :]
    lt = i[:, None] < i[None, :]
    c['m_incl_T'] = (same & le).astype(np.float32)
    c['m_incl_T_u'] = (same & le).astype(np.uint32)
    c['m_strict'] = (same & lt.T).astype(np.float32)
    c['m_same'] = same.astype(np.float32)
    c['tri_strict'] = lt.astype(np.float32)
    c['ones'] = np.ones((128, 128), np.float32)
    blk = np.zeros((128, 128), np.float32)
    blk[:64, :64] = 1
    blk[64:, 64:] = 1
    c['ones_bd'] = blk
    SEG = 512
    rm = np.ones((128, SEG), np.float32)
    rm[:, ::64] = 0
    c['resetmask'] = rm
    ca = np.zeros((128, SEG), np.float32)
    cb = np.zeros((128, SEG), np.float32)
    for ch in range(SEG // 64):
        (ca if ch % 2 == 0 else cb)[:, ch * 64:(ch + 1) * 64] = 1
    c['colA'] = ca
    c['colB'] = cb
    tt_ = np.arange(SEG) % 64
    c['mH0'] = np.broadcast_to((tt_ < 32).astype(np.float32), (128, SEG)).copy()
    c['mH1'] = np.broadcast_to((tt_ >= 32).astype(np.float32), (128, SEG)).copy()
    H = 4
    log_g = np.log1p(-np.exp2(-5.0 - np.arange(H, dtype=np.float64)))
    dt = np.zeros((128, H, 128), np.float64)
    for h in range(H):
        dd = (i[None, :] - i[:, None]).astype(np.float64)
        dt[:, h, :] = np.where(same & le, np.exp(dd * log_g[h]) * 0.125, 0.0)
    c['ret_DT'] = dt.astype(np.float32)
    tmod = i % 64
    qw = np.zeros((128, 2, 128), np.float64)
    for h in range(H):
        hp = (h % 2) * 64
        qw[hp:hp + 64, h // 2, :] = np.exp(log_g[h] * (tmod + 1.0))[None, :]
    qa = qw.copy()
    qa[:, :, 64:] = 0
    qb = qw.copy()
    qb[:, :, :64] = 0
    c['ret_qwA'] = qa.astype(np.float32)
    c['ret_qwB'] = qb.astype(np.float32)
    kw = np.zeros((128, H), np.float64)
    for h in range(H):
        kw[:, h] = np.exp(log_g[h] * (63.0 - tmod)) * 0.125
    c['ret_kw'] = kw.astype(np.float32)
    g64 = np.zeros((128, 2), np.float64)
    for h in range(H):
        hp = (h % 2) * 64
        g64[hp:hp + 64, h // 2] = np.exp(64.0 * log_g[h])
    c['ret_g64'] = g64.astype(np.float32)
    half = 32
    inv = (10000.0 ** (-np.linspace(0.0, 1.0, half, dtype=np.float32))).astype(np.float32)
    ang = (np.arange(pos0, pos0 + T, dtype=np.float32)[:, None] * inv[None, :]).astype(np.float32)
    c['cos'] = np.cos(ang.astype(np.float64)).astype(np.float32)
    c['sin'] = np.sin(ang.astype(np.float64)).astype(np.float32)
    jg = np.zeros((128, n_blk, NE), np.float32)
    jg[:] = (np.arange(n_blk, dtype=np.float32) * EB)[None, :, None]
    c['jgrid'] = jg
    ib = np.zeros((128, 8), np.float32)
    ib[:] = np.arange(8)[None, :] * 128 + np.arange(128)[:, None]
    c['idxbase'] = ib
    return c


CONST_SHAPES = None


class KB:
    def __init__(self, T, dbg=False, stop_after=None, depth=DEPTH, small_w=False):
        self.small_w = small_w
        self.T = T
        self.dbg = dbg
        self.nT = T // 128
        self.NK = T * 4
        self.n_blk = self.NK // EB + NE
        self.PS = self.n_blk * EB
        self.stop_after = stop_after
        self.depth = depth
        self.nc = bass.Bass("TRN2", target_bir_lowering=False)
        self.build()

    def din(self, name, shape, dt=F32):
        return self.nc.dram_tensor(name, list(shape), dt, kind="ExternalInput").ap()

    def dscr(self, name, shape, dt=F32, out=False):
        kind = "ExternalOutput" if (out or self.dbg) else "Internal"
        return self.nc.dram_tensor(name, list(shape), dt, kind=kind).ap()

    def alloc(self, name, cols, dt=F32):
        off = self.aoff
        self.aoff += cols
        assert self.aoff <= self.ACOLS, (name, self.aoff)
        ap = self.arena[:, off:off + cols]
        if dt != F32:
            ap = ap.bitcast(dt)
        return ap

    def mm(self, out, lhsT, rhs, start, stop, R, W, fence=False):
        if fence:
            self.P.fence('pe')
        self.P.op('pe', lambda e: e.matmul(out, lhsT=lhsT, rhs=rhs, start=start, stop=stop), reads=R, writes=W)

    def tr(self, out, in_, R, W):
        ident = self.ident[0:in_.shape[0], 0:in_.shape[0]]
        self.P.op('pe', lambda e: e.transpose(out=out, in_=in_, identity=ident), reads=list(R) + ['const'], writes=W)

    def act(self, out, in_, func, R, W, bias=None, scale=None, accum=None, eng='act'):
        kw = {}
        if bias is not None:
            kw['bias'] = bias
        if scale is not None:
            kw['scale'] = scale
        if accum is not None:
            kw['accum_out'] = accum
        self.P.op('act', lambda e: e.activation(out=out, in_=in_, func=func, **kw), reads=R, writes=W)

    def tt(self, eng, out, in0, in1, op, R, W):
        self.P.op(eng, lambda e: e.tensor_tensor(out=out, in0=in0, in1=in1, op=op), reads=R, writes=W)

    def ts(self, eng, out, in0, s1, s2, op0, op1, R, W):
        if op1 is None:
            self.P.op(eng, lambda e: e.tensor_scalar(out=out, in0=in0, scalar1=s1, scalar2=None, op0=op0), reads=R, writes=W)
        else:
            self.P.op(eng, lambda e: e.tensor_scalar(out=out, in0=in0, scalar1=s1, scalar2=s2, op0=op0, op1=op1), reads=R, writes=W)

    def stt(self, out, in0, scalar, in1, op0, op1, R, W, eng='dve'):
        self.P.op(eng, lambda e: e.scalar_tensor_tensor(out=out, in0=in0, scalar=scalar, in1=in1, op0=op0, op1=op1), reads=R, writes=W)

    def cp(self, eng, out, in_, R, W):
        if eng == 'act':
            self.P.op('act', lambda e: e.copy(out=out, in_=in_), reads=R, writes=W)
        else:
            self.P.op(eng, lambda e: e.tensor_copy(out=out, in_=in_), reads=R, writes=W)

    def memset(self, eng, out, val, W):
        self.P.op(eng, lambda e: e.memset(out, val), reads=(), writes=W)

    def dma(self, eng, out, in_, R, W):
        self.P.dma(eng, lambda e: e.dma_start(out=out, in_=in_), reads=R, writes=W)

    def dma_nc(self, eng, out, in_, R, W):
        self.P.dma(eng, lambda e: e.dma_start(out=out, in_=in_, allow_slow_non_contiguous=True), reads=R, writes=W)

    def gather(self, out, src, idx, R, W):
        self.P.dma('pool', lambda e: e.indirect_dma_start(out=out, out_offset=None, in_=src,
                                                         in_offset=bass.IndirectOffsetOnAxis(ap=idx, axis=0)),
                   reads=R, writes=W)

    def scatter(self, dst, src, idx, R, W):
        self.P.dma('pool', lambda e: e.indirect_dma_start(out=dst, out_offset=bass.IndirectOffsetOnAxis(ap=idx, axis=0),
                                                         in_=src, in_offset=None),
                   reads=R, writes=W)

    def rstd_from_ss(self, ss, n, R):
        pass

    def build(self):
        nc = self.nc
        T, nT = self.T, self.nT
        self.x_in = self.din("x", [T, D])
        self.cT = self.din("cT", [128, 8])
        self.ada_w = self.din("ada_w", [DEPTH, D, 6 * D])
        self.ada_b = self.din("ada_b", [DEPTH, 6 * D])
        self.norm1_g = self.din("norm1_g", [DEPTH, D])
        self.norm2_g = self.din("norm2_g", [DEPTH, D])
        self.w_in = self.din("w_in", [DEPTH, D, N_IN])
        self.w_out = self.din("w_out", [DEPTH, D, D])
        self.hg_lb = self.din("hg_lb_logits", [DEPTH, 512])
        self.mixg = self.din("mixg", [DEPTH, D])
        self.conv_w = self.din("gdn_conv_w", [DEPTH, 4, 768])
        self.A_log = self.din("gdn_A_log", [DEPTH, 4])
        self.dt_bias = self.din("gdn_dt_bias", [DEPTH, 4])
        self.router_w = self.din("router_w", [DEPTH, D, NE])
        self.router_b = self.din("router_b", [DEPTH, NE])
        nrow = 128 if self.small_w else DEPTH * NE * D
        self.w1 = self.din("exp_w1", [nrow, 2 * D])
        self.b1 = self.din("exp_b1", [DEPTH * NE, 2 * D])
        self.w2 = self.din("exp_w2", [nrow, D])
        self.b2 = self.din("exp_b2", [DEPTH * NE, D])
        self.fin_g = self.din("final_norm_g", [D])
        cs = make_consts(128, 0, self.n_blk)
        self.cd = {}
        for k, v in cs.items():
            shp = list(v.shape)
            if k in ('cos', 'sin'):
                shp = [T, 32]
            self.cd[k] = self.din("c_" + k, shp, U32 if v.dtype == np.uint32 else F32)
        self.mod_d = self.dscr("mod_d", [DEPTH, 6 * D])
        self.zT_d = self.dscr("zT_d", [1800, T])
        self.zK_d = self.dscr("zK_d", [T, 2312])
        self.o_d = self.dscr("o_d", [T, D])
        self.gcrow_d = self.dscr("gcrow_d", [4, T])
        self.xa_d = self.dscr("xa_d", [T, D])
        self.xb_d = self.dscr("xb_d", [T, D])
        self.hn_d = self.dscr("hn_d", [T, D])
        self.xg_d = self.dscr("xg_d", [self.PS, D])
        self.yg_d = self.dscr("yg_d", [self.PS, D])
        self.out_d = self.nc.dram_tensor("out", [T, D], F32, kind="ExternalOutput").ap()

        with ExitStack() as st:
            self.P = Prog(nc, st)
            self.ACOLS = 48000
            self.arena = st.enter_context(nc.sbuf_tensor("arena", [128, self.ACOLS], F32))
            self.pb = [st.enter_context(nc.psum_tensor("pb%d" % i, [128, 512], F32)) for i in range(8)]
            self.aoff = 0
            self.ident = self.alloc("ident", 128)
            self.dma('sp', self.ident, self.cd['ident'], [], ['const'])
            self.amark = self.aoff
            self.P.barrier()
            self.prologue()
            xin = self.x_in
            bufs = [self.xa_d, self.xb_d]
            for l in range(self.depth):
                self.m1(l, xin)
                if self.stop_after == ('m1', l):
                    break
                self.m2(l)
                self.m3(l)
                self.m4(l)
                if self.stop_after == ('m4', l):
                    break
                self.m5(l, xin, bufs[0])
                if self.stop_after == ('m5', l):
                    break
                self.moe(l, bufs[0], bufs[1], last=(l == self.depth - 1))
                if self.stop_after == ('moe', l):
                    break
                xin = bufs[1]
            self.P.barrier()
            self.P.emit()
            self.n_inst = self.P.n_inst

    def phase_end(self):
        self.P.barrier()
        self.aoff = self.amark

    def prologue(self):
        condT = self.alloc("condT", 8)
        self.dma('sp', condT, self.cT, [], ['condT'])
        self.act(condT, condT, AF.Silu, ['condT'], ['condT'])
        row = self.alloc("modrow", 6 * D)
        brow = self.alloc("brow", 6 * D)
        aw = [self.alloc("aw0", 8 * 512), self.alloc("aw1", 8 * 512)]
        for l in range(DEPTH):
            self.dma('act', brow[0:1, :], self.ada_b[l:l + 1, :], [], ['brow'])
            for cgi in range(12):
                b = aw[cgi % 2]
                bk = 'aw%d' % (cgi % 2)
                b3 = b.rearrange("p (k n) -> p k n", n=512)
                self.dma('sp', b3, self.ada_w[l][:, cgi * 512:(cgi + 1) * 512].rearrange("(k p) n -> p k n", p=128), [], [bk])
                ps = self.pb[cgi % 2]
                for k in range(8):
                    self.mm(ps[0:1, :], condT[:, k:k + 1], b3[:, k, :], k == 0, k == 7, ['condT', bk], ['pb%d' % (cgi % 2)])
                self.tt('dve', row[0:1, cgi * 512:(cgi + 1) * 512], ps[0:1, :], brow[0:1, cgi * 512:(cgi + 1) * 512], ALU.add,
                        ['brow'], ['modrow', 'pb%d' % (cgi % 2)])
            self.dma('sp', self.mod_d[l:l + 1, :], row[0:1, :], ['modrow'], ['mod_d'])
        self.phase_end()

    def load_mod(self, l, which, gsrc):
        G = self.alloc("G", D)
        S = self.alloc("S", D)
        tmp = self.alloc("Gtmp", D)
        base = 0 if which == 1 else 3 * D
        self.dma('sp', G, gsrc.partition_broadcast(128), [], ['G'])
        self.dma('act', tmp, self.mod_d[l, base + D:base + 2 * D].partition_broadcast(128), [], ['Gtmp'])
        self.dma('sp', S, self.mod_d[l, base:base + D].partition_broadcast(128), [], ['S'])
        self.stt(G, tmp, 1.0, G, ALU.add, ALU.mult, ['G', 'Gtmp'], ['G'])
        return G, S

    def norm_tile(self, src_rows, G, S, xt, hn, ss, tag, eps=1e-6):
        self.dma('sp', xt, src_rows, [], [tag + 'x'])
        self.act(hn, xt, AF.Square, [tag + 'x'], [tag + 'hn', tag + 'ss'], accum=ss)
        self.ts('dve', ss, ss, 1.0 / D, eps, ALU.mult, ALU.add, [tag + 'ss'], [tag + 'ss'])
        self.act(ss, ss, AF.Sqrt, [tag + 'ss'], [tag + 'ss'])
        self.P.op('dve', lambda e: e.reciprocal(out=ss, in_=ss), reads=[tag + 'ss'], writes=[tag + 'ss'])
        self.stt(hn, xt, ss, G, ALU.mult, ALU.mult, [tag + 'x', tag + 'ss', 'G', tag + 'hn'], [tag + 'hn'])
        if S is not None:
            self.tt('pool', hn, hn, S, ALU.add, [tag + 'hn', 'S'], [tag + 'hn'])

    def transpose_tile(self, hn, hnT3, col0, tag, tkey, evac=('act', 'dve')):
        for half in range(2):
            ps = self.pb[6 + half]
            pk = 'pb%d' % (6 + half)
            ps3 = ps.rearrange("p (j n) -> p j n", n=128)
            for j in range(4):
                k = half * 4 + j
                self.tr(ps3[:, j, :], hn[:, k * 128:(k + 1) * 128], [tag + 'hn'], [pk])
            self.cp(evac[half], hnT3[:, half * 4:(half + 1) * 4, col0:col0 + 128], ps3, [], [tkey, pk])

    def m1(self, l, xin):
        T, nT = self.T, self.nT
        G, S = self.load_mod(l, 1, self.norm1_g[l])
        win = self.w_in[l].rearrange("(k p) n -> p k n", p=128)
        mark = self.aoff
        WF = self.alloc("WF", 8 * 1800).rearrange("p (k n) -> p k n", n=1800)
        self.dma('sp', WF[:, :, 0:1024], win[:, :, 0:1024], [], ['WF'])
        self.dma('act', WF[:, :, 1024:1792], win[:, :, 3072:3840], [], ['WF'])
        self.dma('sp', WF[:, :, 1792:1800], win[:, :, 4096:4104], [], ['WF'])
        NB = 512 if T >= 512 else T
        xt = [self.alloc("xt0", D), self.alloc("xt1", D)]
        hn = [self.alloc("hn0", D), self.alloc("hn1", D)]
        ss = [self.alloc("ss0", 1), self.alloc("ss1", 1)]
        hnT = [self.alloc("hnTa", 8 * NB).rearrange("p (k n) -> p k n", n=NB),
               self.alloc("hnTb", 8 * NB).rearrange("p (k n) -> p k n", n=NB)]
        stg = [self.alloc("stg%d" % i, NB) for i in range(4)]
        npb = NB // 128
        nblk = T // NB

        def normF(blk, j):
            it_ = blk * npb + j
            self.norm_tile(xin[it_ * 128:(it_ + 1) * 128, :], G, S, xt[it_ % 2], hn[it_ % 2], ss[it_ % 2], 't%d' % (it_ % 2))

        def trF(blk, j):
            it_ = blk * npb + j
            self.transpose_tile(hn[it_ % 2], hnT[blk % 2], j * 128, 't%d' % (it_ % 2), 'hnT%d' % (blk % 2))

        for j in range(npb):
            normF(0, j)
            trF(0, j)
        nslots = {3 * j: j for j in range(npb)}
        tslots = {3 * j + 2: j for j in range(npb)}
        for blk in range(nblk):
            hT = hnT[blk % 2]
            hk = 'hnT%d' % (blk % 2)
            for mt in range(15):
                msz = 128 if mt < 14 else 8
                ps = self.pb[mt % 4]
                pk = 'pb%d' % (mt % 4)
                for k in range(8):
                    self.mm(ps[0:msz, 0:NB], WF[:, k, mt * 128:mt * 128 + msz], hT[:, k, :], k == 0, k == 7, ['WF', hk], [pk])
                sg = stg[mt % 4]
                sk = 'stg%d' % (mt % 4)
                self.cp('act' if mt % 2 == 0 else 'dve', sg[0:msz, :], ps[0:msz, 0:NB], [], [sk, pk])
                self.dma('sp' if mt % 2 == 0 else 'act', self.zT_d[mt * 128:mt * 128 + msz, blk * NB:(blk + 1) * NB], sg[0:msz, :], [sk], ['zT_d'])
                if blk + 1 < nblk and mt in nslots:
                    normF(blk + 1, nslots[mt])
                if blk + 1 < nblk and mt in tslots:
                    trF(blk + 1, tslots[mt])
        self.P.barrier()
        self.aoff = mark
        WK = self.alloc("WK", 8 * 2312).rearrange("p (k n) -> p k n", n=2312)
        self.dma('sp', WK[:, :, 0:1024], win[:, :, 1024:2048], [], ['WK'])
        self.dma('act', WK[:, :, 1024:2048], win[:, :, 2048:3072], [], ['WK'])
        self.dma('sp', WK[:, :, 2048:2312], win[:, :, 3840:4104], [], ['WK'])
        xt = [self.alloc("xt0", D), self.alloc("xt1", D)]
        hn = [self.alloc("hn0", D), self.alloc("hn1", D)]
        ss = [self.alloc("ss0", 1), self.alloc("ss1", 1)]
        hnT = [self.alloc("hnTa", 8 * 128).rearrange("p (k n) -> p k n", n=128),
               self.alloc("hnTb", 8 * 128).rearrange("p (k n) -> p k n", n=128)]
        zst = [self.alloc("zst0", 2312), self.alloc("zst1", 2312)]
        chunks = [(0, 512), (512, 1024), (1024, 1536), (1536, 2048), (2048, 2312)]
        def normK(i):
            self.norm_tile(xin[i * 128:(i + 1) * 128, :], G, S, xt[i % 2], hn[i % 2], ss[i % 2], 't%d' % (i % 2))

        def trK(i):
            self.transpose_tile(hn[i % 2], hnT[i % 2], 0, 't%d' % (i % 2), 'hnT%d' % (i % 2))

        normK(0)
        trK(0)
        for i in range(nT):
            hk = 'hnT%d' % (i % 2)
            for ci, (c0, c1) in enumerate(chunks):
                ps = self.pb[ci % 4]
                pk = 'pb%d' % (ci % 4)
                for k in range(8):
                    self.mm(ps[:, 0:c1 - c0], hnT[i % 2][:, k, :], WK[:, k, c0:c1], k == 0, k == 7, [hk, 'WK'], [pk])
                self.cp('act' if ci % 2 == 0 else 'dve', zst[i % 2][:, c0:c1], ps[:, 0:c1 - c0], [], ['zst%d' % (i % 2), pk])
                if ci == 0 and i + 1 < nT:
                    normK(i + 1)
                if ci == 3 and i + 1 < nT:
                    trK(i + 1)
            self.dma('sp', self.zK_d[i * 128:(i + 1) * 128, :], zst[i % 2], ['zst%d' % (i % 2)], ['zK_d'])
        self.phase_end()


def prep_inputs(inp, b, T, pos0, n_blk, s0=0, small_w=False):
    m = {}
    m["x"] = np.ascontiguousarray(inp["x"][b, s0:s0 + T])
    m["cT"] = np.ascontiguousarray(inp["c"][b].reshape(8, 128).T)
    for k in ["ada_w", "ada_b", "norm1_g", "norm2_g", "w_in", "w_out", "gdn_conv_w", "gdn_A_log", "gdn_dt_bias",
              "router_w", "router_b", "final_norm_g"]:
        m[k] = np.ascontiguousarray(inp[k])
    m["hg_lb_logits"] = np.ascontiguousarray(inp["hg_lb_logits"])
    m["mixg"] = np.ascontiguousarray(np.concatenate([inp["hg_norm_g"].reshape(DEPTH, 512), inp["ret_norm_g"].reshape(DEPTH, 256),
                                                      inp["gdn_norm_g"].reshape(DEPTH, 256)], axis=1))
    perm = np.concatenate([np.arange(0, 512), np.arange(1024, 1536), np.arange(512, 1024), np.arange(1536, 2048)])
    m["exp_w1"] = np.ascontiguousarray(inp["exp_w1"].reshape(DEPTH * NE * D, 2 * D)[:, perm])
    m["exp_b1"] = np.ascontiguousarray(inp["exp_b1"].reshape(DEPTH * NE, 2 * D)[:, perm])
    m["exp_w2"] = np.ascontiguousarray(inp["exp_w2"]).reshape(DEPTH * NE * D, D)
    m["exp_b2"] = np.ascontiguousarray(inp["exp_b2"]).reshape(DEPTH * NE, D)
    if small_w:
        m["exp_w1"] = m["exp_w1"][:128]
        m["exp_w2"] = m["exp_w2"][:128]
    cs = make_consts(T, pos0, n_blk)
    for k, v in cs.items():
        m["c_" + k] = v
    return m


def _m2(self, l):
    T = self.T
    SEG = min(512, T)
    nseg = T // SEG
    tps = SEG // 128
    nch = SEG // 64
    A = self.alloc
    rmask = A("rmask", SEG); colA = A("colA", SEG); colB = A("colB", SEG)
    mF = A("mF", 128)
    mH0 = A("mH0", SEG); mH1 = A("mH1", SEG)
    self.dma('sp', rmask, self.cd['resetmask'][:, 0:SEG], [], ['const2'])
    self.dma('sp', colA, self.cd['colA'][:, 0:SEG], [], ['const2'])
    self.dma('act', colB, self.cd['colB'][:, 0:SEG], [], ['const2'])
    self.dma('act', mF, self.cd['m_incl_T'], [], ['const2'])
    self.dma('sp', mH0, self.cd['mH0'][:, 0:SEG], [], ['const2'])
    self.dma('act', mH1, self.cd['mH1'][:, 0:SEG], [], ['const2'])
    lbv = A("lbv", 4); omlb = A("omlb", 4); lx0 = A("lx0", 4)
    if l == 0:
        self.memset('dve', lbv, 0.0, ['lb'])
        self.memset('dve', omlb, 1.0, ['lb'])
    else:
        self.dma_nc('sp', lx0, self.hg_lb[0].rearrange("(h d) -> d h", d=128), [], ['lx0'])
        self.dma_nc('sp', lbv, self.hg_lb[1].rearrange("(h d) -> d h", d=128), [], ['lb'])
        self.tt('dve', lbv, lbv, lx0, ALU.subtract, ['lb', 'lx0'], ['lb'])
        self.act(lbv, lbv, AF.Sigmoid, ['lb'], ['lb'])
        self.ts('dve', omlb, lbv, -1.0, 1.0, ALU.mult, ALU.add, ['lb'], ['lb'])
    Sst = A("Sst", 512).rearrange("p (h e) -> p h e", e=128)
    self.memset('pool', Sst, 0.0, ['S0', 'S1', 'S2', 'S3'])
    sTs = [A("sT%d" % i, 128) for i in range(4)]
    for i in range(4):
        self.memset('pool', sTs[i], 0.0, ['sT%d' % i])
    Vt = A("Vt", tps * 512).rearrange("p (t n) -> p t n", n=512)
    ost = [A("ost%d" % i, 128) for i in range(4)]
    names = ['q', 'f', 'lf', 'k', 'b', 'd', 'e', 'e3', 'Qi', 'Ki', 'QdA', 'QdB', 'Kd', 'Qd', 'QdX', 'Ki0']
    sets = []
    for h in range(4):
        s = {n: A("%s%d" % (n, h), SEG) for n in names}
        s['KdTok'] = A("KdTok%d" % h, tps * 128).rearrange("p (t n) -> p t n", n=128)
        sets.append(s)
    for sg in range(nseg):
        c0 = sg * SEG
        self.dma('sp', Vt, self.zK_d[c0:c0 + SEG, 0:512].rearrange("(t p) n -> p t n", p=128), [], ['Vt'])
        for h in range(4):
            s = sets[h]
            K = lambda n: "%s%d" % (n, h)
            self.dma('sp', s['q'], self.zT_d[h * 128:(h + 1) * 128, c0:c0 + SEG], [], [K('q')])
            self.dma('act', s['f'], self.zT_d[512 + h * 128:512 + (h + 1) * 128, c0:c0 + SEG], [], [K('f')])
            self.act(s['q'], s['q'], AF.Silu, [K('q')], [K('q')])
            self.act(s['f'], s['f'], AF.Sigmoid, [K('f')], [K('f')])
            self.ts('dve', s['f'], s['f'], omlb[:, h:h + 1], lbv[:, h:h + 1], ALU.mult, ALU.add, [K('f'), 'lb'], [K('f')])
            self.act(s['lf'], s['f'], AF.Ln, [K('f')], [K('lf')])
            self.ts('pool', s['k'], s['f'], -1.0, 1.0, ALU.mult, ALU.add, [K('f')], [K('k')])
            self.P.op('dve', lambda e, s=s: e.tensor_tensor_scan(out=s['b'], data0=rmask, data1=s['lf'], initial=0.0, op0=ALU.mult, op1=ALU.add),
                      reads=[K('lf'), 'const2'], writes=[K('b')])
            b3 = s['b'].rearrange("p (c t) -> p c t", t=64)
            d3 = s['d'].rearrange("p (c t) -> p c t", t=64)
            self.tt('dve', d3, b3, b3[:, :, 31:32].to_broadcast([128, nch, 64]), ALU.subtract, [K('b')], [K('d')])
            self.ts('dve', s['d'], s['d'], -80.0, 80.0, ALU.max, ALU.min, [K('d')], [K('d')])
            self.act(s['e'], s['d'], AF.Exp, [K('d')], [K('e')])
            self.tt('dve', s['Qi'], s['q'], s['e'], ALU.mult, [K('q'), K('e')], [K('Qi')])
            self.tt('pool', s['Qi'], s['Qi'], mH1, ALU.mult, [K('Qi'), 'const2'], [K('Qi')])
            self.act(s['e'], s['d'], AF.Exp, [K('d')], [K('e')], scale=-1.0)
            self.tt('pool', s['Ki'], s['k'], s['e'], ALU.mult, [K('k'), K('e')], [K('Ki')])
            self.act(s['e3'], s['b'], AF.Exp, [K('b')], [K('e3')])
            self.tt('pool', s['Qd'], s['q'], s['e3'], ALU.mult, [K('q'), K('e3')], [K('Qd')])
            self.tt('pool', s['QdA'], s['Qd'], colA, ALU.mult, [K('Qd'), 'const2'], [K('QdA')])
            self.tt('pool', s['QdB'], s['Qd'], colB, ALU.mult, [K('Qd'), 'const2'], [K('QdB')])
            self.tt('pool', s['QdX'], s['Qd'], mH0, ALU.mult, [K('Qd'), 'const2'], [K('QdX')])
            self.tt('dve', d3, b3, b3[:, :, 63:64].to_broadcast([128, nch, 64]), ALU.subtract, [K('b')], [K('d')])
            self.act(s['e'], s['d'], AF.Exp, [K('d')], [K('e')], scale=-1.0)
            self.tt('dve', s['Kd'], s['k'], s['e'], ALU.mult, [K('k'), K('e')], [K('Kd')])
            self.ts('dve', s['d'], s['b'], -1.0, 80.0, ALU.mult, ALU.min, [K('b')], [K('d')])
            self.act(s['e'], s['d'], AF.Exp, [K('d')], [K('e')])
            self.tt('dve', s['Ki0'], s['k'], s['e'], ALU.mult, [K('k'), K('e')], [K('Ki0')])
            self.tt('pool', s['Ki0'], s['Ki0'], mH0, ALU.mult, [K('Ki0'), 'const2'], [K('Ki0')])
        for ti in range(tps):
            tk = slice(ti * 128, (ti + 1) * 128)
            row0 = c0 + ti * 128
            BA = lambda h: self.pb[2 * h]
            BB = lambda h: self.pb[2 * h + 1]
            KA = lambda h: 'pb%d' % (2 * h)
            KBk = lambda h: 'pb%d' % (2 * h + 1)
            if int(os.environ.get('M2LVL', '9')) < 1:
                continue
            for h in range(4):
                s = sets[h]
                K = lambda n: "%s%d" % (n, h)
                self.mm(BA(h)[:, 0:128], s['Ki'][:, tk], s['Qi'][:, tk], True, False, [K('Ki'), K('Qi')], [KA(h)])
                self.mm(BA(h)[:, 0:128], s['Ki0'][:, tk], s['QdX'][:, tk], False, True, [K('Ki0'), K('QdX')], [KA(h)])
                self.tr(BA(h)[:, 128:256], s['Kd'][:, tk], [K('Kd')], [KA(h)])
            if int(os.environ.get('M2LVL', '9')) < 2:
                continue
            for h in range(4):
                s = sets[h]
                K = lambda n: "%s%d" % (n, h)
                self.tt('dve', sTs[h], BA(h)[:, 0:128], mF, ALU.mult, ['const2'], ['sT%d' % h, KA(h)])
                self.cp('act', s['KdTok'][:, ti, :], BA(h)[:, 128:256], [], [K('KdTok'), KA(h)])
            if int(os.environ.get('M2LVL', '9')) < 3:
                continue
            for h in range(4):
                s = sets[h]
                K = lambda n: "%s%d" % (n, h)
                vh = Vt[:, ti, h * 128:(h + 1) * 128]
                SUB = os.environ.get('M2SUB', '1234')
                if '1' in SUB:
                    self.mm(BB(h)[:, 0:128], sTs[h], vh, True, False, ['sT%d' % h, 'Vt'], [KBk(h)])
                if '2' in SUB:
                    self.mm(BB(h)[:, 0:128], s['QdA'][:, tk], Sst[:, h, :], False, False, [K('QdA'), 'S%d' % h], [KBk(h)])
                if '3' in SUB:
                    self.mm(BA(h)[:, 256:384], s['KdTok'][0:64, ti, :], Vt[0:64, ti, h * 128:(h + 1) * 128], True, True, [K('KdTok'), 'Vt'], [KA(h)])
                if '4' in SUB:
                    self.mm(BA(h)[:, 384:512], s['KdTok'][64:128, ti, :], Vt[64:128, ti, h * 128:(h + 1) * 128], True, True, [K('KdTok'), 'Vt'], [KA(h)], fence=True)
            if int(os.environ.get('M2LVL', '9')) < 4:
                continue
            for h in range(4):
                s = sets[h]
                K = lambda n: "%s%d" % (n, h)
                cA = (ti * 2) * 64 + 63
                self.stt(Sst[:, h, :], Sst[:, h, :], s['e3'][:, cA:cA + 1], BA(h)[:, 256:384], ALU.mult, ALU.add, ['S%d' % h, K('e3')], ['S%d' % h, KA(h)])
            if int(os.environ.get('M2LVL', '9')) < 5:
                continue
            for h in range(4):
                s = sets[h]
                K = lambda n: "%s%d" % (n, h)
                self.mm(BB(h)[:, 0:128], s['QdB'][:, tk], Sst[:, h, :], False, True, [K('QdB'), 'S%d' % h], [KBk(h)])
            if int(os.environ.get('M2LVL', '9')) < 6:
                continue
            for h in range(4):
                s = sets[h]
                K = lambda n: "%s%d" % (n, h)
                cB = (ti * 2 + 1) * 64 + 63
                self.stt(Sst[:, h, :], Sst[:, h, :], s['e3'][:, cB:cB + 1], BA(h)[:, 384:512], ALU.mult, ALU.add, ['S%d' % h, K('e3')], ['S%d' % h, KA(h)])
                self.cp('act', ost[h], BB(h)[:, 0:128], [], ['ost%d' % h, KBk(h)])
                self.dma('sp' if h % 2 == 0 else 'act', self.o_d[row0:row0 + 128, h * 128:(h + 1) * 128], ost[h], ['ost%d' % h], ['o_d'])
    self.phase_end()


KB.m2 = _m2
KB.m3 = lambda self, l: None
KB.m4 = lambda self, l: None


def _m3(self, l):
    T = self.T
    SEG = min(512, T)
    nseg = T // SEG
    tps = SEG // 128
    A = self.alloc
    DT = A("DT", 512).rearrange("p (h t) -> p h t", t=128)
    qwA = A("qwA", 256).rearrange("p (j t) -> p j t", t=128)
    qwB = A("qwB", 256).rearrange("p (j t) -> p j t", t=128)
    kw = A("kw", 4)
    g64 = A("g64", 2)
    self.dma('sp', DT, self.cd['ret_DT'], [], ['c3'])
    self.dma('sp', qwA, self.cd['ret_qwA'], [], ['c3'])
    self.dma('act', qwB, self.cd['ret_qwB'], [], ['c3'])
    self.dma('act', kw, self.cd['ret_kw'], [], ['c3'])
    self.dma('act', g64, self.cd['ret_g64'], [], ['c3'])
    S2 = A("S2r", 128).rearrange("p (j e) -> p j e", e=64)
    self.memset('pool', S2, 0.0, ['Sr0', 'Sr1'])
    qk = A("qk", tps * 512).rearrange("p (t n) -> p t n", n=512)
    rv = A("rv", tps * 256).rearrange("p (t n) -> p t n", n=256)
    cs = A("cs", tps * 32).rearrange("p (t n) -> p t n", n=32)
    sn = A("sn", tps * 32).rearrange("p (t n) -> p t n", n=32)
    NB = 2
    rot = [A("rot%d" % i, 512) for i in range(NB)]
    ta = [A("ta%d" % i, 256) for i in range(NB)]
    tb = [A("tb%d" % i, 256) for i in range(NB)]
    tc_ = [A("tc%d" % i, 256) for i in range(NB)]
    td = [A("td%d" % i, 256) for i in range(NB)]
    Kwp = [A("Kwp%d" % i, 512) for i in range(NB)]
    for i in range(NB):
        self.memset('pool', Kwp[i], 0.0, ['Kwp%d' % i])
    qT = [A("qT%d" % i, 256).rearrange("p (j t) -> p j t", t=128) for i in range(NB)]
    kT = [A("kT%d" % i, 256).rearrange("p (j t) -> p j t", t=128) for i in range(NB)]
    QA = [A("QA%d" % i, 256).rearrange("p (j t) -> p j t", t=128) for i in range(NB)]
    QB = [A("QB%d" % i, 256).rearrange("p (j t) -> p j t", t=128) for i in range(NB)]
    sTs = [A("rsT%d" % i, 128) for i in range(4)]
    ost = [A("rost%d" % i, 256) for i in range(NB)]
    it = 0
    for sg in range(nseg):
        c0 = sg * SEG
        self.dma('sp', qk, self.zK_d[c0:c0 + SEG, 1024:1536].rearrange("(t p) n -> p t n", p=128), [], ['qk'])
        self.dma('act', rv, self.zK_d[c0:c0 + SEG, 1536:1792].rearrange("(t p) n -> p t n", p=128), [], ['rv'])
        self.dma('sp', cs, self.cd['cos'][c0:c0 + SEG, :].rearrange("(t p) n -> p t n", p=128), [], ['cs'])
        self.dma('act', sn, self.cd['sin'][c0:c0 + SEG, :].rearrange("(t p) n -> p t n", p=128), [], ['sn'])
        for ti in range(tps):
            b = it % NB
            it += 1
            bk = lambda n: "%s%d" % (n, b)
            row0 = c0 + ti * 128
            v4 = qk[:, ti, :].rearrange("p (g two r) -> p g two r", g=8, two=2)
            t1 = v4[:, :, 0, :]
            t2 = v4[:, :, 1, :]
            o4 = rot[b].rearrange("p (g two r) -> p g two r", g=8, two=2)
            cb = cs[:, ti, :].unsqueeze(1).to_broadcast([128, 8, 32])
            sb_ = sn[:, ti, :].unsqueeze(1).to_broadcast([128, 8, 32])
            a3 = lambda x: x.rearrange("p (g r) -> p g r", r=32)
            self.tt('dve', a3(ta[b]), t1, cb, ALU.mult, ['qk', 'cs'], [bk('ta')])
            self.tt('dve', a3(tb[b]), t2, sb_, ALU.mult, ['qk', 'sn'], [bk('tb')])
            self.tt('dve', o4[:, :, 0, :], a3(ta[b]), a3(tb[b]), ALU.subtract, [bk('ta'), bk('tb')], [bk('rot')])
            self.tt('pool', a3(tc_[b]), t1, sb_, ALU.mult, ['qk', 'sn'], [bk('tc')])
            self.tt('pool', a3(td[b]), t2, cb, ALU.mult, ['qk', 'cs'], [bk('td')])
            self.tt('pool', o4[:, :, 1, :], a3(tc_[b]), a3(td[b]), ALU.add, [bk('tc'), bk('td')], [bk('rot')])
            kp = rot[b][:, 256:512].rearrange("p (j i d) -> p j i d", j=2, i=2)
            Kw4 = Kwp[b].rearrange("p (j i c) -> p j i c", j=2, i=2)
            kw3 = kw.rearrange("p (j i) -> p j i", i=2)
            for i in range(2):
                self.tt('dve' if i == 0 else 'pool', Kw4[:, :, i, i * 64:(i + 1) * 64], kp[:, :, i, :],
                        kw3[:, :, i:i + 1].to_broadcast([128, 2, 64]), ALU.mult, [bk('rot'), 'c3'], [bk('Kwp')])
            p0 = self.pb[0].rearrange("p (r t) -> p r t", t=128)
            for r in range(4):
                self.tr(p0[:, r, :], rot[b][:, r * 128:(r + 1) * 128], [bk('rot')], ['pb0'])
            self.cp('act', qT[b], p0[:, 0:2, :], [], [bk('qT'), 'pb0'])
            self.cp('act', kT[b], p0[:, 2:4, :], [], [bk('kT'), 'pb0'])
            self.tt('dve', QA[b], p0[:, 0:2, :], qwA, ALU.mult, ['c3'], [bk('QA'), 'pb0'])
            self.tt('dve', QB[b], p0[:, 0:2, :], qwB, ALU.mult, ['c3'], [bk('QB'), 'pb0'])
            for h in range(4):
                j, hp = h // 2, (h % 2) * 64
                bank = 1 + (h % 2)
                self.mm(self.pb[bank][:, j * 128:(j + 1) * 128], kT[b][hp:hp + 64, j, :], qT[b][hp:hp + 64, j, :], True, True,
                        [bk('kT'), bk('qT')], ['pb%d' % bank])
            for h in range(4):
                j = h // 2
                bank = 1 + (h % 2)
                self.tt('dve', sTs[h], self.pb[bank][:, j * 128:(j + 1) * 128], DT[:, h, :], ALU.mult, ['c3'], ['rsT%d' % h, 'pb%d' % bank])
            for h in range(4):
                j, hp = h // 2, (h % 2) * 64
                ob = self.pb[3 + h][:, 0:64]
                self.mm(ob, sTs[h], rv[:, ti, h * 64:(h + 1) * 64], True, False, ['rsT%d' % h, 'rv'], ['pb%d' % (3 + h)])
                self.mm(ob, QA[b][hp:hp + 64, j, :], S2[hp:hp + 64, j, :], False, False, [bk('QA'), 'Sr%d' % j], ['pb%d' % (3 + h)])
            for j in range(2):
                kvb = self.pb[7][:, j * 64:(j + 1) * 64]
                for i in range(2):
                    h = 2 * j + i
                    self.mm(kvb, Kwp[b][0:64, h * 128:(h + 1) * 128], rv[0:64, ti, h * 64:(h + 1) * 64], i == 0, i == 1,
                            [bk('Kwp'), 'rv'], ['pb7'])
            for j in range(2):
                self.stt(S2[:, j, :], S2[:, j, :], g64[:, j:j + 1], self.pb[7][:, j * 64:(j + 1) * 64], ALU.mult, ALU.add,
                         ['Sr%d' % j, 'c3'], ['Sr%d' % j, 'pb7'])
            for h in range(4):
                j, hp = h // 2, (h % 2) * 64
                ob = self.pb[3 + h][:, 0:64]
                self.mm(ob, QB[b][hp:hp + 64, j, :], S2[hp:hp + 64, j, :], False, True, [bk('QB'), 'Sr%d' % j], ['pb%d' % (3 + h)])
            for j in range(2):
                kvb = self.pb[0][:, j * 64:(j + 1) * 64]
                for i in range(2):
                    h = 2 * j + i
                    self.mm(kvb, Kwp[b][64:128, h * 128:(h + 1) * 128], rv[64:128, ti, h * 64:(h + 1) * 64], i == 0, i == 1,
                            [bk('Kwp'), 'rv'], ['pb0'], fence=(i == 0 and j == 0))
            for j in range(2):
                self.stt(S2[:, j, :], S2[:, j, :], g64[:, j:j + 1], self.pb[0][:, j * 64:(j + 1) * 64], ALU.mult, ALU.add,
                         ['Sr%d' % j, 'c3'], ['Sr%d' % j, 'pb0'])
            for h in range(4):
                self.cp('act', ost[b][:, h * 64:(h + 1) * 64], self.pb[3 + h][:, 0:64], [], [bk('rost'), 'pb%d' % (3 + h)])
            self.dma('sp', self.o_d[row0:row0 + 128, 512:768], ost[b], [bk('rost')], ['o_d'])
    self.phase_end()


KB.m3 = _m3


def _m4(self, l):
    T = self.T
    SEG = min(512, T)
    nseg = T // SEG
    tps = SEG // 128
    A = self.alloc
    C = 'c4'
    m_strict = A("m_strict", 128); m_inclT = A("m_inclT", 128); m_same = A("m_same", 128); ones_bd = A("ones_bd", 128)
    rmask = A("rmask4", SEG); colA = A("colA4", SEG); colB = A("colB4", SEG)
    for ap, nm in [(m_strict, 'm_strict'), (m_inclT, 'm_incl_T'), (m_same, 'm_same'), (ones_bd, 'ones_bd')]:
        self.dma('sp', ap, self.cd[nm], [], [C])
    self.dma('act', rmask, self.cd['resetmask'][:, 0:SEG], [], [C])
    self.dma('act', colA, self.cd['colA'][:, 0:SEG], [], [C])
    self.dma('act', colB, self.cd['colB'][:, 0:SEG], [], [C])
    cwf = A("cw", 24)
    for jj in range(4):
        self.dma_nc('sp', cwf[:, jj * 6:(jj + 1) * 6], self.conv_w[l][jj].rearrange("(t p) -> p t", p=128), [], [C])
    cwv = lambda ct, jj: cwf[:, jj * 6 + ct:jj * 6 + ct + 1]
    dtb4 = A("dtb4", 1); negA4 = A("negA4", 1); dtbB = A("dtbB", 4); negAB = A("negAB", 4)
    self.dma_nc('sp', dtb4[0:4, :], self.dt_bias[l].rearrange("(h o) -> h o", o=1), [], [C])
    self.dma_nc('sp', negA4[0:4, :], self.A_log[l].rearrange("(h o) -> h o", o=1), [], ['negA4'])
    self.act(negA4[0:4, :], negA4[0:4, :], AF.Exp, ['negA4'], ['negA4'])
    self.ts('dve', negA4[0:4, :], negA4[0:4, :], -1.0, None, ALU.mult, None, ['negA4'], ['negA4'])
    self.dma('act', dtbB, self.dt_bias[l].partition_broadcast(128), [], [C])
    self.dma('act', negAB, self.A_log[l].partition_broadcast(128), [], ['negAB'])
    self.act(negAB, negAB, AF.Exp, ['negAB'], ['negAB'])
    self.ts('dve', negAB, negAB, -1.0, None, ALU.mult, None, ['negAB'], ['negAB'])
    S2 = A("S2g", 128).rearrange("p (j e) -> p j e", e=64)
    self.memset('pool', S2, 0.0, ['Sg0', 'Sg1'])
    qnT = A("qnT", 2 * SEG).rearrange("p (j t) -> p j t", t=SEG)
    knT = A("knT", 2 * SEG).rearrange("p (j t) -> p j t", t=SEG)
    vT = A("vT", 2 * SEG).rearrange("p (j t) -> p j t", t=SEG)
    Gbc = A("Gbc", 4 * SEG).rearrange("p (h t) -> p h t", t=SEG)
    EGs = A("EGs", 2 * SEG).rearrange("p (j t) -> p j t", t=SEG)
    qd = A("qd", 2 * SEG).rearrange("p (j t) -> p j t", t=SEG)
    qdA = A("qdA", 2 * SEG).rearrange("p (j t) -> p j t", t=SEG)
    qdB = A("qdB", 2 * SEG).rearrange("p (j t) -> p j t", t=SEG)
    u = A("u", SEG + 3); acc = A("acc", SEG); xs = A("xs", SEG); sq = A("sq", SEG); rn = A("rn", SEG)
    arow = A("arow", SEG); grow = A("grow", SEG)
    ab = A("ab", tps * 8).rearrange("p (t n) -> p t n", n=8)
    gtm = A("gtm", tps * 4).rearrange("p (t n) -> p t n", n=4)
    gcT = A("gcT", tps * 4).rearrange("p (t n) -> p t n", n=4)
    glT = A("glT", tps * 4).rearrange("p (t n) -> p t n", n=4)
    beta = A("beta", tps * 4).rearrange("p (t n) -> p t n", n=4)
    nbeta = A("nbeta", tps * 4).rearrange("p (t n) -> p t n", n=4)
    bke = A("bke", tps * 4).rearrange("p (t n) -> p t n", n=4)
    kds = A("kds", tps * 4).rearrange("p (t n) -> p t n", n=4)
    ktok = A("ktok", 256); vtok = A("vtok", 256)
    Y = [A("Y%d" % i, 512).rearrange("p (h c) -> p h c", c=128) for i in range(2)]
    Pm = [A("Pm%d" % i, 512).rearrange("p (h c) -> p h c", c=128) for i in range(2)]
    PT = [A("PT%d" % i, 512).rearrange("p (h c) -> p h c", c=128) for i in range(2)]
    kdpad = A("kdpad", 512)
    self.memset('pool', kdpad, 0.0, ['kdpad'])
    A1 = A("A1", 512).rearrange("p (h c) -> p h c", c=128)
    A2 = A("A2", 512).rearrange("p (h c) -> p h c", c=128)
    tmp = A("tmp4", 512).rearrange("p (h c) -> p h c", c=128)
    tmp2 = A("tmp42", 512).rearrange("p (h c) -> p h c", c=128)
    qkTm = A("qkTm", 512).rearrange("p (h c) -> p h c", c=128)
    kcc = A("kcc", 256)
    kcTA = A("kcTA", 256).rearrange("p (j t) -> p j t", t=128)
    kcTB = A("kcTB", 256).rearrange("p (j t) -> p j t", t=128)
    self.memset('pool', kcTA, 0.0, ['kcTA'])
    self.memset('pool', kcTB, 0.0, ['kcTB'])
    vnew = A("vnew", 256).rearrange("p (h e) -> p h e", e=64)
    ost = A("gost", 256)
    par = lambda x3, i: x3.rearrange("p (j i) c -> p j i c", i=2)[:, :, i, :]
    for sg in range(nseg):
        c0 = sg * SEG
        for ct in range(6):
            r0 = 1024 + ct * 128
            if sg == 0:
                self.memset('pool', u[:, 0:3], 0.0, ['u'])
            else:
                self.dma_nc('act', u[:, 0:3], self.zT_d[r0:r0 + 128, c0 - 3:c0], [], ['u'])
            self.dma('sp', u[:, 3:SEG + 3], self.zT_d[r0:r0 + 128, c0:c0 + SEG], [], ['u'])
            self.ts('dve', acc, u[:, 3:SEG + 3], cwv(ct, 3), None, ALU.mult, None, ['u', C], ['acc'])
            for jj in (2, 1, 0):
                self.stt(acc, u[:, jj:jj + SEG], cwv(ct, jj), acc, ALU.mult, ALU.add, ['u', C, 'acc'], ['acc'])
            if ct >= 4:
                self.act(vT[:, ct - 4, :], acc, AF.Silu, ['acc'], ['vT'])
                continue
            self.act(xs, acc, AF.Silu, ['acc'], ['xs'])
            self.tt('pool', sq, xs, xs, ALU.mult, ['xs'], ['sq'])
            bank = ct % 2
            self.mm(self.pb[bank][:, 0:SEG], ones_bd, sq, True, True, [C, 'sq'], ['pb%d' % bank])
            self.act(rn, self.pb[bank][:, 0:SEG], AF.Sqrt, [], ['rn', 'pb%d' % bank], bias=1e-6)
            self.P.op('dve', lambda e: e.reciprocal(out=rn, in_=rn), reads=['rn'], writes=['rn'])
            dst = qnT[:, ct, :] if ct < 2 else knT[:, ct - 2, :]
            self.stt(dst, xs, 0.125 if ct < 2 else 1.0, rn, ALU.mult, ALU.mult, ['xs', 'rn'], ['qnT' if ct < 2 else 'knT'])
        self.dma('sp', arow[0:4, :], self.zT_d[1792:1796, c0:c0 + SEG], [], ['arow'])
        self.act(arow[0:4, :], arow[0:4, :], AF.Exp, ['arow', C], ['arow'], bias=dtb4[0:4, :])
        self.act(arow[0:4, :], arow[0:4, :], AF.Ln, ['arow'], ['arow'], bias=1.0)
        self.ts('dve', arow[0:4, :], arow[0:4, :], negA4[0:4, :], None, ALU.mult, None, ['arow', 'negA4'], ['arow'])
        self.P.op('dve', lambda e: e.tensor_tensor_scan(out=grow[0:4, :], data0=rmask[0:4, :], data1=arow[0:4, :], initial=0.0,
                                                       op0=ALU.mult, op1=ALU.add), reads=['arow', C], writes=['grow'])
        self.dma('sp', self.gcrow_d[0:4, c0:c0 + SEG], grow[0:4, :], ['grow'], ['gcrow_d'])
        for h in range(4):
            self.dma('sp' if h % 2 == 0 else 'act', Gbc[:, h, :], self.gcrow_d[h, c0:c0 + SEG].partition_broadcast(128), ['gcrow_d'], ['Gbc'])
        for j in range(2):
            for i in range(2):
                self.act(EGs[i * 64:(i + 1) * 64, j, :], Gbc[i * 64:(i + 1) * 64, 2 * j + i, :], AF.Exp, ['Gbc'], ['EGs'])
        self.tt('dve', qd, qnT, EGs, ALU.mult, ['qnT', 'EGs'], ['qd'])
        self.tt('pool', qdA, qd, colA.unsqueeze(1).to_broadcast([128, 2, SEG]), ALU.mult, ['qd', C], ['qdA'])
        self.tt('pool', qdB, qd, colB.unsqueeze(1).to_broadcast([128, 2, SEG]), ALU.mult, ['qd', C], ['qdB'])
        self.dma_nc('sp', ab, self.zK_d[c0:c0 + SEG, 2304:2312].rearrange("(t p) n -> p t n", p=128), [], ['ab'])
        self.tt('dve', gtm, ab[:, :, 0:4], dtbB.unsqueeze(1).to_broadcast([128, tps, 4]), ALU.add, ['ab', C], ['gtm'])
        self.act(gtm, gtm, AF.Exp, ['gtm'], ['gtm'])
        self.act(gtm, gtm, AF.Ln, ['gtm'], ['gtm'], bias=1.0)
        self.tt('dve', gtm, gtm, negAB.unsqueeze(1).to_broadcast([128, tps, 4]), ALU.mult, ['gtm', 'negAB'], ['gtm'])
        g2 = gtm.rearrange("p t n -> p (t n)")
        self.mm(self.pb[2][:, 0:tps * 4], m_inclT, g2, True, True, [C, 'gtm'], ['pb2'])
        self.mm(self.pb[3][:, 0:tps * 4], m_same, g2, True, True, [C, 'gtm'], ['pb3'])
        self.cp('dve', gcT.rearrange("p t n -> p (t n)"), self.pb[2][:, 0:tps * 4], [], ['gcT', 'pb2'])
        self.cp('dve', glT.rearrange("p t n -> p (t n)"), self.pb[3][:, 0:tps * 4], [], ['glT', 'pb3'])
        self.act(beta, ab[:, :, 4:8], AF.Sigmoid, ['ab'], ['beta'])
        self.ts('dve', nbeta, beta, -1.0, None, ALU.mult, None, ['beta'], ['nbeta'])
        self.act(bke, gcT, AF.Exp, ['gcT'], ['bke'])
        self.tt('dve', bke, bke, beta, ALU.mult, ['bke', 'beta'], ['bke'])
        self.tt('dve', kds, glT, gcT, ALU.subtract, ['glT', 'gcT'], ['kds'])
        self.act(kds, kds, AF.Exp, ['kds'], ['kds'])
        for ti in range(tps):
            tk = slice(ti * 128, (ti + 1) * 128)
            row0 = c0 + ti * 128
            p0 = self.pb[0].rearrange("p (r t) -> p r t", t=128)
            for j in range(2):
                self.tr(p0[:, j, :], knT[:, j, tk], ['knT'], ['pb0'])
                self.tr(p0[:, 2 + j, :], vT[:, j, tk], ['vT'], ['pb0'])
            self.cp('act', ktok, self.pb[0][:, 0:256], [], ['ktok', 'pb0'])
            self.cp('act', vtok, self.pb[0][:, 256:512], [], ['vtok', 'pb0'])
            Y0 = Y[0]
            self.tt('dve', Y0[:, :, 0:64], vtok.rearrange("p (h d) -> p h d", d=64), beta[:, ti, :].unsqueeze(2).to_broadcast([128, 4, 64]),
                    ALU.mult, ['vtok', 'beta'], ['Y0'])
            self.tt('dve', Y0[:, :, 64:128], ktok.rearrange("p (h d) -> p h d", d=64), bke[:, ti, :].unsqueeze(2).to_broadcast([128, 4, 64]),
                    ALU.mult, ['ktok', 'bke'], ['Y0'])
            k4 = ktok.rearrange("p (j i d) -> p j i d", j=2, i=2)
            Kd4 = kdpad.rearrange("p (j i c) -> p j i c", j=2, i=2)
            kds3 = kds[:, ti, :].rearrange("p (j i) -> p j i", i=2)
            for i in range(2):
                self.tt('pool', Kd4[:, :, i, i * 64:(i + 1) * 64], k4[:, :, i, :], kds3[:, :, i:i + 1].to_broadcast([128, 2, 64]), ALU.mult,
                        ['ktok', 'kds'], ['kdpad'])
            for h in range(4):
                j, i = h // 2, h % 2
                hp = i * 64
                self.mm(self.pb[1 + i][:, j * 128:(j + 1) * 128], knT[hp:hp + 64, j, tk], knT[hp:hp + 64, j, tk], True, True, ['knT'], ['pb%d' % (1 + i)])
                self.mm(self.pb[3 + i][:, j * 128:(j + 1) * 128], knT[hp:hp + 64, j, tk], qnT[hp:hp + 64, j, tk], True, True, ['knT', 'qnT'], ['pb%d' % (3 + i)])
            for h in range(4):
                self.ts('dve', A1[:, h, :], Gbc[:, h, tk], gcT[:, ti, h:h + 1], 0.0, ALU.subtract, ALU.max, ['Gbc', 'gcT'], ['A1'])
                self.ts('dve', A2[:, h, :], Gbc[:, h, tk], gcT[:, ti, h:h + 1], 0.0, ALU.subtract, ALU.min, ['Gbc', 'gcT'], ['A2'])
            self.act(A1, A1, AF.Exp, ['A1'], ['A1'], scale=-1.0)
            self.act(A2, A2, AF.Exp, ['A2'], ['A2'])
            for i in range(2):
                g3 = self.pb[1 + i][:, 0:256].rearrange("p (j c) -> p j c", c=128)
                self.tt('dve', par(tmp, i), g3, par(A1, i), ALU.mult, ['A1'], ['tmp4', 'pb%d' % (1 + i)])
                q3 = self.pb[3 + i][:, 0:256].rearrange("p (j c) -> p j c", c=128)
                self.tt('dve', par(tmp2, i), q3, par(A2, i), ALU.mult, ['A2'], ['tmp42', 'pb%d' % (3 + i)])
            P0 = Pm[0]
            for h in range(4):
                self.stt(P0[:, h, :], tmp[:, h, :], nbeta[:, ti, h:h + 1], m_strict, ALU.mult, ALU.mult, ['tmp4', 'nbeta', C], ['Pm0'])
            self.tt('pool', qkTm, tmp2, m_inclT.unsqueeze(1).to_broadcast([128, 4, 128]), ALU.mult, ['tmp42', C], ['qkTm'])
            p5 = self.pb[5].rearrange("p (r t) -> p r t", t=128)
            for h in range(4):
                self.tr(p5[:, h, :], P0[:, h, :], ['Pm0'], ['pb5'])
            self.cp('act', PT[0], p5, [], ['PT0', 'pb5'])
            cur = 0
            for lev in range(6):
                nxt = 1 - cur
                p6 = self.pb[6].rearrange("p (r t) -> p r t", t=128)
                for h in range(4):
                    self.mm(p6[:, h, :], PT[cur][:, h, :], Y[cur][:, h, :], True, True, ['PT%d' % cur, 'Y%d' % cur], ['pb6'])
                self.tt('dve', Y[nxt], Y[cur], p6, ALU.add, ['Y%d' % cur], ['Y%d' % nxt, 'pb6'])
                if lev < 5:
                    p7 = self.pb[7].rearrange("p (r t) -> p r t", t=128)
                    for h in range(4):
                        self.mm(p7[:, h, :], PT[cur][:, h, :], Pm[cur][:, h, :], True, True, ['PT%d' % cur, 'Pm%d' % cur], ['pb7'])
                    for h in range(4):
                        self.mm(p5[:, h, :], Pm[cur][:, h, :], PT[cur][:, h, :], True, True, ['PT%d' % cur, 'Pm%d' % cur], ['pb5'])
                    self.cp('act', Pm[nxt], p7, [], ['Pm%d' % nxt, 'pb7'])
                    self.cp('act', PT[nxt], p5, [], ['PT%d' % nxt, 'pb5'])
                cur = nxt
            Yf = Y[cur]
            YK = 'Y%d' % cur
            self.cp('pool', kcc.rearrange("p (h d) -> p h d", d=64), Yf[:, :, 64:128], [YK], ['kcc'])
            for j in range(2):
                self.tr(p0[:, j, :], kcc[:, j * 128:(j + 1) * 128], ['kcc'], ['pb0'])
            self.cp('act', kcTA[:, :, 0:64], p0[:, 0:2, 0:64], [], ['kcTA', 'pb0'])
            self.cp('act', kcTB[:, :, 64:128], p0[:, 0:2, 64:128], [], ['kcTB', 'pb0'])
            for h in range(4):
                j, i = h // 2, h % 2
                hp = i * 64
                self.mm(self.pb[4 + h][:, 0:64], qdA[hp:hp + 64, j, tk], S2[hp:hp + 64, j, :], True, False, ['qdA', 'Sg%d' % j], ['pb%d' % (4 + h)])
                self.mm(self.pb[1 + i][:, j * 64:(j + 1) * 64], kcTA[hp:hp + 64, j, :], S2[hp:hp + 64, j, :], True, True, ['kcTA', 'Sg%d' % j], ['pb%d' % (1 + i)])
            for i in range(2):
                v3 = self.pb[1 + i][0:64, 0:128].rearrange("p (j e) -> p j e", e=64)
                self.tt('dve', par(vnew, i)[0:64], par(Yf, i)[0:64, :, 0:64], v3, ALU.subtract, [YK], ['vnew', 'pb%d' % (1 + i)])
            for j in range(2):
                for i in range(2):
                    h = 2 * j + i
                    self.mm(self.pb[3][:, j * 64:(j + 1) * 64], kdpad[0:64, h * 128:(h + 1) * 128], vnew[0:64, h, :], i == 0, i == 1,
                            ['kdpad', 'vnew'], ['pb3'])
            for j in range(2):
                cA = (ti * 2) * 64 + 63
                self.stt(S2[:, j, :], S2[:, j, :], EGs[:, j, cA:cA + 1], self.pb[3][:, j * 64:(j + 1) * 64], ALU.mult, ALU.add,
                         ['Sg%d' % j, 'EGs'], ['Sg%d' % j, 'pb3'])
            for h in range(4):
                j, i = h // 2, h % 2
                hp = i * 64
                self.mm(self.pb[4 + h][:, 0:64], qdB[hp:hp + 64, j, tk], S2[hp:hp + 64, j, :], False, False, ['qdB', 'Sg%d' % j], ['pb%d' % (4 + h)])
                self.mm(self.pb[1 + i][:, j * 64:(j + 1) * 64], kcTB[hp:hp + 64, j, :], S2[hp:hp + 64, j, :], True, True, ['kcTB', 'Sg%d' % j], ['pb%d' % (1 + i)])
            for i in range(2):
                v3 = self.pb[1 + i][64:128, 0:128].rearrange("p (j e) -> p j e", e=64)
                self.tt('dve', par(vnew, i)[64:128], par(Yf, i)[64:128, :, 0:64], v3, ALU.subtract, [YK], ['vnew', 'pb%d' % (1 + i)])
            for h in range(4):
                self.mm(self.pb[4 + h][:, 0:64], qkTm[:, h, :], vnew[:, h, :], False, True, ['qkTm', 'vnew'], ['pb%d' % (4 + h)])
            for j in range(2):
                for i in range(2):
                    h = 2 * j + i
                    self.mm(self.pb[3][:, 128 + j * 64:128 + (j + 1) * 64], kdpad[64:128, h * 128:(h + 1) * 128], vnew[64:128, h, :], i == 0, i == 1,
                            ['kdpad', 'vnew'], ['pb3'], fence=(i == 0 and j == 0))
            for j in range(2):
                cB = (ti * 2 + 1) * 64 + 63
                self.stt(S2[:, j, :], S2[:, j, :], EGs[:, j, cB:cB + 1], self.pb[3][:, 128 + j * 64:128 + (j + 1) * 64], ALU.mult, ALU.add,
                         ['Sg%d' % j, 'EGs'], ['Sg%d' % j, 'pb3'])
            for h in range(4):
                self.cp('act', ost[:, h * 64:(h + 1) * 64], self.pb[4 + h][:, 0:64], [], ['gost', 'pb%d' % (4 + h)])
            self.dma('sp', self.o_d[row0:row0 + 128, 768:1024], ost, ['gost'], ['o_d'])
    self.phase_end()


KB.m4 = _m4


def _m5(self, l, xin, xout):
    T, nT = self.T, self.nT
    A = self.alloc
    Wo = A("Wo", 8 * D).rearrange("p (k n) -> p k n", n=D)
    self.dma('sp', Wo, self.w_out[l].rearrange("(k p) n -> p k n", p=128), [], ['Wo'])
    gbc = A("gbc", D); gt1 = A("gt1", D)
    self.dma('act', gbc, self.mixg[l].partition_broadcast(128), [], ['c5'])
    self.dma('act', gt1, self.mod_d[l, 2 * D:3 * D].partition_broadcast(128), [], ['c5'])
    NB = 2
    ot = [A("ot%d" % i, D) for i in range(NB)]
    gtile = [A("gtile%d" % i, D) for i in range(NB)]
    xt = [A("x5%d" % i, D) for i in range(NB)]
    sq = [A("sq5%d" % i, D) for i in range(NB)]
    ss = [A("ss5%d" % i, 12) for i in range(NB)]
    on = [A("on%d" % i, D) for i in range(NB)]
    onT = [A("onT%d" % i, 8 * 128).rearrange("p (k n) -> p k n", n=128) for i in range(NB)]
    yst = [A("y5%d" % i, D) for i in range(NB)]
    def prep5(i):
        b = i % NB
        bk = lambda n: "%s%d" % (n, b)
        r = slice(i * 128, (i + 1) * 128)
        self.dma('sp', ot[b], self.o_d[r, :], [], [bk('ot')])
        self.dma('act', gtile[b][:, 0:512], self.zK_d[r, 512:1024], [], [bk('gtile')])
        self.dma('act', gtile[b][:, 512:1024], self.zK_d[r, 1792:2304], [], [bk('gtile')])
        self.dma('sp', xt[b], xin[r, :], [], [bk('x5')])
        self.tt('pool', sq[b], ot[b], ot[b], ALU.mult, [bk('ot')], [bk('sq5')])
        self.P.op('dve', lambda e, b=b: e.tensor_reduce(out=ss[b][:, 0:4], in_=sq[b][:, 0:512].rearrange("p (h d) -> p h d", d=128), axis=AX.X, op=ALU.add),
                  reads=[bk('sq5')], writes=[bk('ss5')])
        self.P.op('dve', lambda e, b=b: e.tensor_reduce(out=ss[b][:, 4:12], in_=sq[b][:, 512:1024].rearrange("p (h d) -> p h d", d=64), axis=AX.X, op=ALU.add),
                  reads=[bk('sq5')], writes=[bk('ss5')])
        self.ts('dve', ss[b][:, 0:4], ss[b][:, 0:4], 1.0 / 128, 1e-6, ALU.mult, ALU.add, [bk('ss5')], [bk('ss5')])
        self.ts('dve', ss[b][:, 4:12], ss[b][:, 4:12], 1.0 / 64, 1e-6, ALU.mult, ALU.add, [bk('ss5')], [bk('ss5')])
        self.act(ss[b], ss[b], AF.Sqrt, [bk('ss5')], [bk('ss5')])
        self.P.op('dve', lambda e, b=b: e.reciprocal(out=ss[b], in_=ss[b]), reads=[bk('ss5')], writes=[bk('ss5')])
        self.tt('dve', on[b][:, 0:512].rearrange("p (h d) -> p h d", d=128), ot[b][:, 0:512].rearrange("p (h d) -> p h d", d=128),
                ss[b][:, 0:4].unsqueeze(2).to_broadcast([128, 4, 128]), ALU.mult, [bk('ot'), bk('ss5')], [bk('on') + 'hn'])
        self.tt('dve', on[b][:, 512:1024].rearrange("p (h d) -> p h d", d=64), ot[b][:, 512:1024].rearrange("p (h d) -> p h d", d=64),
                ss[b][:, 4:12].unsqueeze(2).to_broadcast([128, 8, 64]), ALU.mult, [bk('ot'), bk('ss5')], [bk('on') + 'hn'])
        self.act(gtile[b], gtile[b], AF.Silu, [bk('gtile')], [bk('gtile')])
        self.tt('pool', on[b], on[b], gbc, ALU.mult, [bk('on') + 'hn', 'c5'], [bk('on') + 'hn'])
        self.tt('dve', on[b], on[b], gtile[b], ALU.mult, [bk('on') + 'hn', bk('gtile')], [bk('on') + 'hn'])


    def tr5(i):
        b = i % NB
        bk = lambda n: "%s%d" % (n, b)
        self.transpose_tile(on[b], onT[b], 0, bk('on'), bk('onT'))

    prep5(0)
    tr5(0)
    for i in range(nT):
        b = i % NB
        bk = lambda n: "%s%d" % (n, b)
        r = slice(i * 128, (i + 1) * 128)
        for half in range(2):
            ps = self.pb[half][:, :]
            for k in range(8):
                self.mm(ps, onT[b][:, k, :], Wo[:, k, half * 512:(half + 1) * 512], k == 0, k == 7, [bk('onT'), 'Wo'], ['pb%d' % half])
            self.tt('dve', yst[b][:, half * 512:(half + 1) * 512], ps, gt1[:, half * 512:(half + 1) * 512], ALU.mult, ['c5'], [bk('y5'), 'pb%d' % half])
            if half == 0 and i + 1 < nT:
                prep5(i + 1)
            if half == 1 and i + 1 < nT:
                tr5(i + 1)
        self.tt('pool', yst[b], yst[b], xt[b], ALU.add, [bk('y5'), bk('x5')], [bk('y5')])
        self.dma('sp', xout[r, :], yst[b], [bk('y5')], ['xout'])
    self.phase_end()


KB.m5 = _m5
KB.moe = lambda self, l, xin, xout, last: None


def _moe(self, l, xin, xout, last):
    T, nT, n_blk = self.T, self.nT, self.n_blk
    A = self.alloc
    gsel = A("gsel", nT * 4).rearrange("p (i k) -> p i k", k=4)
    sloti = A("sloti", nT * 4, I32).rearrange("p (i k) -> p i k", k=4)
    idxW = A("idxW", n_blk * 8, I32).rearrange("p (j k) -> p j k", k=8)
    bidx = A("bidx", n_blk, I32)
    idxH = A("idxH", n_blk * 16, I32).rearrange("p (j k q) -> p j k q", k=8, q=2)
    mark2 = self.aoff
    zt = A("zt", 8192)
    self.memset('pool', zt, 0.0, ['zt'])
    zrows = 128 * 8
    for zi in range(self.PS // zrows):
        self.dma('sp' if zi % 2 == 0 else 'act', self.xg_d[zi * zrows:(zi + 1) * zrows, :].rearrange("(p a) n -> p (a n)", a=8), zt, ['zt'], ['xg_d'])
    G, S = self.load_mod(l, 2, self.norm2_g[l])
    Wr = A("Wr", 8 * NE).rearrange("p (k n) -> p k n", n=NE)
    self.dma('sp', Wr, self.router_w[l].rearrange("(k p) n -> p k n", p=128), [], ['c6'])
    rb = A("rb", NE)
    self.dma('act', rb, self.router_b[l].partition_broadcast(128), [], ['c6'])
    ones = A("ones6", 128); tri = A("tri6", 128)
    self.dma('sp', ones, self.cd['ones'], [], ['c6'])
    self.dma('act', tri, self.cd['tri_strict'], [], ['c6'])
    maskAll = A("maskAll", nT * NE).rearrange("p (i e) -> p i e", e=NE)
    gateAll = A("gateAll", nT * NE).rearrange("p (i e) -> p i e", e=NE)
    NB = 2
    xt = [A("x6%d" % i, D) for i in range(NB)]
    hn = [A("h6%d" % i, D) for i in range(NB)]
    ss = [A("s6%d" % i, 1) for i in range(NB)]
    hnT = [A("hT6%d" % i, 8 * 128).rearrange("p (k n) -> p k n", n=128) for i in range(NB)]
    lg = [A("lg%d" % i, NE) for i in range(NB)]
    ex = [A("ex%d" % i, NE) for i in range(NB)]
    m8 = [A("m8%d" % i, 8) for i in range(NB)]
    sm = [A("sm%d" % i, 2) for i in range(NB)]
    def normE(i):
        b = i % NB
        tg = 'u%d' % b
        r = slice(i * 128, (i + 1) * 128)
        self.norm_tile(xin[r, :], G, S, xt[b], hn[b], ss[b], tg)
        self.dma('act', self.hn_d[r, :], hn[b], [tg + 'hn'], ['hn_d'])

    normE(0)
    for i in range(nT):
        b = i % NB
        bk = lambda n: "%s%d" % (n, b)
        tg = 'u%d' % b
        r = slice(i * 128, (i + 1) * 128)
        self.transpose_tile(hn[b], hnT[b], 0, tg, bk('hT6'))
        ps = self.pb[b][:, 0:NE]
        for k in range(8):
            self.mm(ps, hnT[b][:, k, :], Wr[:, k, :], k == 0, k == 7, [bk('hT6'), 'c6'], ['pb%d' % b])
        if i + 1 < nT:
            normE(i + 1)
        self.tt('dve', lg[b], ps, rb, ALU.add, ['c6'], [bk('lg'), 'pb%d' % b])
        self.P.op('dve', lambda e, b=b: e.max(out=m8[b], in_=lg[b]), reads=[bk('lg')], writes=[bk('m8')])
        self.ts('dve', maskAll[:, i, :], lg[b], m8[b][:, 3:4], None, ALU.is_ge, None, [bk('lg'), bk('m8')], ['maskAll'])
        self.ts('dve', sm[b][:, 0:1], m8[b][:, 0:1], -1.0, None, ALU.mult, None, [bk('m8')], [bk('sm')])
        self.act(ex[b], lg[b], AF.Exp, [bk('lg'), bk('sm')], [bk('ex')], bias=sm[b][:, 0:1])
        self.tt('dve', ex[b], ex[b], maskAll[:, i, :], ALU.mult, [bk('ex'), 'maskAll'], [bk('ex')])
        self.P.op('dve', lambda e, b=b: e.tensor_reduce(out=sm[b][:, 1:2], in_=ex[b], axis=AX.X, op=ALU.add), reads=[bk('ex')], writes=[bk('sm')])
        self.P.op('dve', lambda e, b=b: e.reciprocal(out=sm[b][:, 1:2], in_=sm[b][:, 1:2]), reads=[bk('sm')], writes=[bk('sm')])
        self.ts('dve', gateAll[:, i, :], ex[b], sm[b][:, 1:2], None, ALU.mult, None, [bk('ex'), bk('sm')], ['gateAll'])
    tcnt = A("tcnt", nT * NE).rearrange("p (i e) -> p i e", e=NE)
    rank = A("rank", nT * NE).rearrange("p (i e) -> p i e", e=NE)
    mflat = maskAll.rearrange("p i e -> p (i e)")
    CH = 512
    ncol = nT * NE
    for c in range((ncol + CH - 1) // CH):
        a0, a1 = c * CH, min(ncol, (c + 1) * CH)
        self.mm(self.pb[2][:, 0:a1 - a0], ones, mflat[:, a0:a1], True, True, ['c6', 'maskAll'], ['pb2'])
        self.cp('act', tcnt.rearrange("p i e -> p (i e)")[:, a0:a1], self.pb[2][:, 0:a1 - a0], [], ['tcnt', 'pb2'])
        self.mm(self.pb[3][:, 0:a1 - a0], tri, mflat[:, a0:a1], True, True, ['c6', 'maskAll'], ['pb3'])
        self.cp('dve', rank.rearrange("p i e -> p (i e)")[:, a0:a1], self.pb[3][:, 0:a1 - a0], [], ['rank', 'pb3'])
    tcT = A("tcT", NE * nT).rearrange("p (e i) -> p e i", i=nT)
    incl = A("incl", NE * nT).rearrange("p (e i) -> p e i", i=nT)
    rm2 = A("rm2", NE * nT).rearrange("p (e i) -> p e i", i=nT)
    self.cp('dve', tcT, tcnt.rearrange("p i e -> p e i"), ['tcnt'], ['tcT'])
    self.memset('pool', rm2, 1.0, ['rm2'])
    self.memset('pool', rm2[:, :, 0:1], 0.0, ['rm2'])
    self.P.op('dve', lambda e: e.tensor_tensor_scan(out=incl.rearrange("p e i -> p (e i)"), data0=rm2.rearrange("p e i -> p (e i)"),
                                                   data1=tcT.rearrange("p e i -> p (e i)"), initial=0.0, op0=ALU.mult, op1=ALU.add),
              reads=['tcT', 'rm2'], writes=['incl'])
    tot = A("tot", NE); pad = A("pad", NE); padi = A("padi", NE, I32); pend = A("pend", NE); pst = A("pst", NE); one32 = A("one32", NE)
    self.cp('dve', tot, incl[:, :, nT - 1], ['incl'], ['tot'])
    self.ts('dve', pad, tot, float(EB - 1), None, ALU.add, None, ['tot'], ['pad'])
    self.cp('dve', padi, pad, ['pad'], ['padi'])
    self.ts('dve', padi, padi, 9, 9, ALU.arith_shift_right, ALU.logical_shift_left, ['padi'], ['padi'])
    self.cp('dve', pad, padi, ['padi'], ['pad'])
    self.memset('pool', one32, 1.0, ['one32'])
    self.P.op('dve', lambda e: e.tensor_tensor_scan(out=pend, data0=one32, data1=pad, initial=0.0, op0=ALU.mult, op1=ALU.add),
              reads=['pad', 'one32'], writes=['pend'])
    self.tt('dve', pst, pend, pad, ALU.subtract, ['pend', 'pad'], ['pst'])
    self.tt('dve', incl, incl, tcT, ALU.subtract, ['incl', 'tcT'], ['incl'])
    self.tt('dve', incl, incl, pst.unsqueeze(2).to_broadcast([128, NE, nT]), ALU.add, ['incl', 'pst'], ['incl'])
    self.tt('dve', rank, rank, incl.rearrange("p e i -> p i e"), ALU.add, ['rank', 'incl'], ['rank'])
    self.stt(rank, rank, 1.0, maskAll, ALU.add, ALU.mult, ['rank', 'maskAll'], ['rank'])
    jg = A("jg", n_blk * NE).rearrange("p (j e) -> p j e", e=NE)
    self.dma('sp', jg, self.cd['jgrid'], [], ['jg'])
    bef = A("bef", n_blk)
    self.tt('dve', jg, pend.unsqueeze(1).to_broadcast([128, n_blk, NE]), jg, ALU.is_le, ['pend', 'jg'], ['jg'])
    self.P.op('dve', lambda e: e.tensor_reduce(out=bef, in_=jg, axis=AX.X, op=ALU.add), reads=['jg'], writes=['bef'])
    self.ts('dve', bef, bef, float(NE - 1), float(l * NE), ALU.min, ALU.add, ['bef'], ['bef'])
    self.cp('dve', bidx, bef, ['bef'], ['bidx'])
    ib = A("ib", 8)
    self.dma('act', ib, self.cd['idxbase'], [], ['ib'])
    idf = A("idf", n_blk * 8).rearrange("p (j k) -> p j k", k=8)
    self.ts('dve', bef, bef, float(D), None, ALU.mult, None, ['bef'], ['bef'])
    self.tt('dve', idf, bef.unsqueeze(2).to_broadcast([128, n_blk, 8]), ib.unsqueeze(1).to_broadcast([128, n_blk, 8]), ALU.add,
            ['bef', 'ib'], ['idf'])
    self.cp('dve', idxW, idf, ['idf'], ['idxW'])
    idq = A("idq", n_blk * 16).rearrange("p (j k q) -> p j k q", k=8, q=2)
    for q in range(2):
        self.ts('dve', idq[:, :, :, q], idf, 2.0, float(q), ALU.mult, ALU.add, ['idf'], ['idq'])
    self.cp('dve', idxH.rearrange("p j k q -> p (j k q)"), idq.rearrange("p j k q -> p (j k q)"), ['idq'], ['idxH'])
    slotf = A("slotf", nT * 4).rearrange("p (i k) -> p i k", k=4)
    oh = [A("oh%d" % i, NE) for i in range(NB)]
    for i in range(nT):
        b = i % NB
        bk = lambda n: "%s%d" % (n, b)
        r = slice(i * 128, (i + 1) * 128)
        self.P.op('dve', lambda e, b=b, i=i: e.max(out=m8[b], in_=rank[:, i, :]), reads=['rank'], writes=[bk('m8')])
        for k in range(4):
            self.ts('dve', oh[b], rank[:, i, :], m8[b][:, k:k + 1], None, ALU.is_equal, None, ['rank', bk('m8')], [bk('oh')])
            self.tt('dve', oh[b], oh[b], gateAll[:, i, :], ALU.mult, [bk('oh'), 'gateAll'], [bk('oh')])
            self.P.op('dve', lambda e, b=b, i=i, k=k: e.tensor_reduce(out=gsel[:, i, k:k + 1], in_=oh[b], axis=AX.X, op=ALU.add),
                      reads=[bk('oh')], writes=['gsel'])
        self.ts('dve', slotf[:, i, :], m8[b][:, 0:4], -1.0, None, ALU.add, None, [bk('m8')], ['slotf'])
        self.cp('dve', sloti[:, i, :], slotf[:, i, :], ['slotf'], ['sloti%d' % i])
        self.dma('sp', hn[b], self.hn_d[r, :], ['hn_d'], [bk('h6')])
        for k in range(4):
            self.scatter(self.xg_d, hn[b], sloti[:, i, k:k + 1], [bk('h6'), 'sloti%d' % i], ['xg_d'])
    self.P.barrier()
    self.aoff = mark2
    w1q = self.w1.rearrange("r (q c) -> (r q) c", q=2)
    W1h = [A("W1A", 8 * 1024).rearrange("p (k n) -> p k n", n=1024), A("W1B", 8 * 1024).rearrange("p (k n) -> p k n", n=1024)]
    W2 = A("W2", 8 * D).rearrange("p (k n) -> p k n", n=D)
    b1bc = A("b1bc", 2 * D); b2bc = A("b2bc", D)
    onesr = A("onesr", 8)
    self.memset('pool', onesr, 1.0, ['onesr'])
    b1col = A("b1col", 16); b1p1 = A("b1p1", 8)
    xg = [A("xg%d" % i, D) for i in range(2)]
    xgT = A("xgT", 8 * EB).rearrange("p (k n) -> p k n", n=EB)
    actT = A("actT", 8 * EB).rearrange("p (k n) -> p k n", n=EB)
    gtl = [A("gtl%d" % i, EB) for i in range(2)]
    sgl = [A("sgl%d" % i, EB) for i in range(2)]
    ltl = [A("ltl%d" % i, EB) for i in range(2)]
    yst = [A("yst%d" % i, 512) for i in range(2)]
    nrt = EB // 128
    for j in range(n_blk):
        for hf in range(2):
            for k in range(8):
                self.gather(W1h[hf][:, k, :], w1q, idxH[:, j, k, hf:hf + 1], ['idxH'], ['W1%d' % hf])
        self.gather(b1bc, self.b1, bidx[:, j:j + 1], ['bidx'], ['b1bc'])
        for k in range(8):
            self.gather(W2[:, k, :], self.w2, idxW[:, j, k:k + 1], ['idxW'], ['W2'])
        self.gather(b2bc, self.b2, bidx[:, j:j + 1], ['bidx'], ['b2bc'])
        for m in range(16):
            mq = m % 8
            bc0 = (mq // 4) * 1024 + (m // 8) * 512 + (mq % 4) * 128
            self.mm(self.pb[5][:, m:m + 1], b1bc[0:1, bc0:bc0 + 128], onesr[0:1, 0:1], True, True, ['b1bc', 'onesr'], ['pb5'])
        self.cp('act', b1col, self.pb[5][:, 0:16], [], ['b1col', 'pb5'])
        self.ts('dve', b1p1, b1col[:, 8:16], 1.0, None, ALU.add, None, ['b1col'], ['b1p1'])
        for rr in range(nrt):
            b = rr % 2
            r0 = j * EB + rr * 128
            self.dma('sp' if rr % 2 == 0 else 'act', xg[b], self.xg_d[r0:r0 + 128, :], ['xg_d'], ['xg%dhn' % b])
            self.transpose_tile(xg[b], xgT, rr * 128, 'xg%d' % b, 'xgT')
        for m in range(8):
            hf, mm_ = m // 4, m % 4
            pp = m % 2
            banks = (0, 1) if pp == 0 else (2, 3)
            for part in range(2):
                bank = banks[part]
                ps = self.pb[bank][:, 0:EB]
                c0 = part * 512 + mm_ * 128
                for k in range(8):
                    self.mm(ps, W1h[hf][:, k, c0:c0 + 128], xgT[:, k, :], k == 0, k == 7, ['W1%d' % hf, 'xgT'], ['pb%d' % bank])
            g_, s_, l_ = gtl[pp], sgl[pp], ltl[pp]
            gk, sk, lk = 'gtl%d' % pp, 'sgl%d' % pp, 'ltl%d' % pp
            self.ts('dve', g_, self.pb[banks[0]][:, 0:EB], b1col[:, m:m + 1], 7.0, ALU.add, ALU.min, ['b1col'], [gk, 'pb%d' % banks[0]])
            self.act(s_, g_, AF.Sigmoid, [gk], [sk], scale=1.702)
            self.ts('dve', l_, self.pb[banks[1]][:, 0:EB], b1p1[:, m:m + 1], -6.0, ALU.add, ALU.max, ['b1p1'], [lk, 'pb%d' % banks[1]])
            self.tt('dve', s_, s_, g_, ALU.mult, [sk, gk], [sk])
            self.stt(actT[:, m, :], l_, 8.0, s_, ALU.min, ALU.mult, [lk, sk], ['actT'])
        for rr in range(nrt):
            r0 = j * EB + rr * 128
            for m in range(8):
                for half in range(2):
                    self.mm(self.pb[6 + half][:, :], actT[:, m, rr * 128:(rr + 1) * 128], W2[:, m, half * 512:(half + 1) * 512], m == 0, m == 7,
                            ['actT', 'W2'], ['pb%d' % (6 + half)])
            for half in range(2):
                self.tt('dve', yst[half], self.pb[6 + half][:, :], b2bc[:, half * 512:(half + 1) * 512], ALU.add, ['b2bc'], ['yst%d' % half, 'pb%d' % (6 + half)])
                self.dma('sp' if half == 0 else 'act', self.yg_d[r0:r0 + 128, half * 512:(half + 1) * 512], yst[half], ['yst%d' % half], ['yg_d'])
    self.P.barrier()
    self.aoff = mark2
    gt2 = A("gt2", D)
    self.dma('act', gt2, self.mod_d[l, 5 * D:6 * D].partition_broadcast(128), [], ['c7'])
    fg = A("fg", D)
    if last:
        self.dma('act', fg, self.fin_g.partition_broadcast(128), [], ['c7'])
    yr = [A("yr%d" % i, D) for i in range(4)]
    accb = [A("acc7%d" % i, D) for i in range(2)]
    x1 = [A("x7%d" % i, D) for i in range(2)]
    ob = [A("o7%d" % i, D) for i in range(2)]
    s7 = [A("s7%d" % i, 1) for i in range(2)]
    for i in range(nT):
        b = i % 2
        bk = lambda n: "%s%d" % (n, b)
        r = slice(i * 128, (i + 1) * 128)
        self.dma('sp', x1[b], xin[r, :], [], [bk('x7')])
        for k in range(4):
            self.gather(yr[k], self.yg_d, sloti[:, i, k:k + 1], ['yg_d'], ['yr%d' % k])
        self.ts('dve', accb[b], yr[0], gsel[:, i, 0:1], None, ALU.mult, None, ['yr0'], [bk('acc7')])
        for k in range(1, 4):
            self.stt(accb[b], yr[k], gsel[:, i, k:k + 1], accb[b], ALU.mult, ALU.add, ['yr%d' % k, bk('acc7')], [bk('acc7')])
        self.tt('dve', accb[b], accb[b], gt2, ALU.mult, [bk('acc7'), 'c7'], [bk('acc7')])
        self.tt('dve', accb[b], accb[b], x1[b], ALU.add, [bk('acc7'), bk('x7')], [bk('acc7')])
        if not last:
            self.dma('sp', xout[r, :], accb[b], [bk('acc7')], ['xout'])
        else:
            if self.dbg:
                self.dma('act', xout[r, :], accb[b], [bk('acc7')], ['xout'])
            self.act(ob[b], accb[b], AF.Square, [bk('acc7')], [bk('o7'), bk('s7')], accum=s7[b])
            self.ts('dve', s7[b], s7[b], 1.0 / D, 1e-6, ALU.mult, ALU.add, [bk('s7')], [bk('s7')])
            self.act(s7[b], s7[b], AF.Sqrt, [bk('s7')], [bk('s7')])
            self.P.op('dve', lambda e, b=b: e.reciprocal(out=s7[b], in_=s7[b]), reads=[bk('s7')], writes=[bk('s7')])
            self.stt(ob[b], accb[b], s7[b], fg, ALU.mult, ALU.mult, [bk('acc7'), bk('s7'), 'c7', bk('o7')], [bk('o7')])
            self.dma('sp', self.out_d[r, :], ob[b], [bk('o7')], ['out'])
    self.phase_end()


KB.moe = _moe


_CACHE = {}


def kernel(**inputs):
    B, S, _ = inputs["x"].shape
    T = S
    if T not in _CACHE:
        _CACHE[T] = KB(T)
    kb = _CACHE[T]
    base = prep_inputs(inputs, 0, T, 0, kb.n_blk)
    in_maps = []
    for b in range(B):
        m = dict(base)
        m["x"] = np.ascontiguousarray(inputs["x"][b])
        m["cT"] = np.ascontiguousarray(inputs["c"][b].reshape(8, 128).T)
        in_maps.append(m)
    res = run_bass_kernel_spmd(kb.nc, in_maps, core_ids=list(range(B)))
    out = np.stack([np.asarray(r["out"], dtype=np.float32) for r in res.results], axis=0)
    return out
```
